# Optimizing a Trainium2 kernel written in Bass

```python
import math
import jax
import jax.numpy as jnp
from jax import lax
import numpy as np

D_MODEL = 2048
BATCH = 8
SEQ = 2048
DEPTH = 4

HEAD_DIM = 128
A_HEADS = 8
A_WIDTH = A_HEADS * HEAD_DIM
A_BRANCHES = ((128, 1), (512, 4), (2048, 16))
SSD_HEADS = 16
SSD_HEAD_DIM = 64
SSD_WIDTH = SSD_HEADS * SSD_HEAD_DIM
SSD_GROUPS = 4
SSD_STATE = 128
SSD_CONV = 4
SSD_CHUNK = 128
SSD_CONV_WIDTH = SSD_WIDTH + 2 * SSD_GROUPS * SSD_STATE
EVEN_IN_WIDTH = 3 * A_WIDTH + SSD_WIDTH + SSD_CONV_WIDTH + SSD_HEADS
MIX_WIDTH = A_WIDTH + SSD_WIDTH
C_HEADS = 16
C_WIDTH = C_HEADS * HEAD_DIM
MOBA_BLOCK = 256
MOBA_TOPK = 3
MOBA_QUERY_CHUNK = 8
FFN_DIM = 5632
N_EXPERTS = 8
TOP_K = 2
LN_EPS = 1e-5
RMS_EPS = 1e-5
DEEPNORM_ALPHA = (2 * DEPTH) ** 0.25
DEEPNORM_BETA = (8 * DEPTH) ** -0.25
N_EVEN = (DEPTH + 1) // 2
N_ODD = DEPTH // 2
NEG = -1e30

kernel_name = 'hybrid_dilated_ssd_moba_moe'


def layer_norm(x, g, b):
    xf = x.astype(jnp.float32)
    mu = jnp.mean(xf, axis=-1, keepdims=True)
    xc = xf - mu
    var = jnp.mean(xc * xc, axis=-1, keepdims=True)
    return (xc * lax.rsqrt(var + LN_EPS) * g + b).astype(x.dtype)


def softmax_stats(s):
    m = jnp.max(s, axis=-1, keepdims=True)
    p = jnp.exp(s - m)
    den = jnp.sum(p, axis=-1, keepdims=True)
    return p / den, (m + jnp.log(den))[..., 0]


def dilated_branch(q, k, v, window, dilation):
    bsz, seq, nh, hd = q.shape
    band = window // dilation
    sub_len = seq // dilation
    n_blk = -(-sub_len // band)
    pad = n_blk * band - sub_len

    def strided(a):
        return a.reshape(bsz, sub_len, dilation, nh, hd).transpose(0, 2, 1, 3, 4)

    qs = jnp.pad(strided(q), ((0, 0), (0, 0), (0, pad), (0, 0), (0, 0)))
    qs = qs.reshape(bsz, dilation, n_blk, band, nh, hd)

    def band_keys(a):
        a = jnp.pad(strided(a), ((0, 0), (0, 0), (band, pad), (0, 0), (0, 0)))
        a = a.reshape(bsz, dilation, n_blk + 1, band, nh, hd)
        return jnp.concatenate([a[:, :, :-1], a[:, :, 1:]], axis=3)

    kb, vb = band_keys(k), band_keys(v)
    s = jnp.einsum('bdnqhe,bdnkhe->bdnhqk', qs, kb).astype(jnp.float32) * (hd ** -0.5)
    qi = jnp.arange(band)[:, None]
    kj = jnp.arange(2 * band)[None, :]
    dist = band + qi - kj
    in_win = (dist >= 0) & (dist <= band)
    key_pos = (jnp.arange(n_blk)[:, None, None] - 1) * band + kj[None]
    mask = in_win[None] & (key_pos >= 0)
    s = jnp.where(mask[None, None, :, None], s, NEG)
    p, lse = softmax_stats(s)
    o = jnp.einsum('bdnhqk,bdnkhe->bdnqhe', p.astype(v.dtype), vb)
    o = o.reshape(bsz, dilation, n_blk * band, nh, hd)[:, :, :sub_len]
    o = o.transpose(0, 2, 1, 3, 4).reshape(bsz, seq, nh, hd)
    lse = lse.transpose(0, 1, 2, 4, 3).reshape(bsz, dilation, n_blk * band, nh)[:, :, :sub_len]
    lse = lse.transpose(0, 2, 1, 3).reshape(bsz, seq, nh)
    return o, lse


def dilated_attention(q, k, v):
    outs, lses = [], []
    for window, dilation in A_BRANCHES:
        o, l = dilated_branch(q, k, v, window, dilation)
        outs.append(o)
        lses.append(l)
    w = jax.nn.softmax(jnp.stack(lses), axis=0)
    o = jnp.einsum('ibsh,ibshe->bshe', w, jnp.stack(outs).astype(jnp.float32))
    bsz, seq = q.shape[:2]
    return o.astype(q.dtype).reshape(bsz, seq, A_WIDTH)


def causal_dwconv(x, w, b):
    ksz, ch = w.shape
    y = lax.conv_general_dilated(x, w[:, None, :], window_strides=(1,),
                                 padding=((ksz - 1, 0),),
                                 dimension_numbers=('NWC', 'WIO', 'NWC'),
                                 feature_group_count=ch)
    return y + b


def ssd_scan(x, dt, a_head, bmat, cmat):
    bsz, seq, nh, hp = x.shape
    ng, ns = bmat.shape[2], bmat.shape[3]
    rep = nh // ng
    nc = seq // SSD_CHUNK
    q = SSD_CHUNK
    xdt = (x * dt[..., None]).reshape(bsz, nc, q, ng, rep, hp)
    a_cs = jnp.cumsum((dt * a_head).reshape(bsz, nc, q, nh), axis=2)
    bc = bmat.reshape(bsz, nc, q, ng, ns)
    cc = cmat.reshape(bsz, nc, q, ng, ns)
    causal = jnp.tril(jnp.ones((q, q), dtype=bool))
    seg = a_cs[:, :, :, None, :] - a_cs[:, :, None, :, :]
    decay_in = jnp.exp(jnp.where(causal[None, None, :, :, None], seg, -jnp.inf))
    decay_in = decay_in.reshape(bsz, nc, q, q, ng, rep)
    cb = jnp.einsum('bclgn,bcsgn->bclsg', cc, bc)
    y_diag = jnp.einsum('bclsgr,bcsgrp->bclgrp', cb[..., None] * decay_in, xdt)
    decay_st = jnp.exp(a_cs[:, :, -1:, :] - a_cs).reshape(bsz, nc, q, ng, rep)
    states = jnp.einsum('bclgn,bclgr,bclgrp->bcgrpn', bc, decay_st, xdt)
    chunk_decay = jnp.exp(a_cs[:, :, -1, :]).reshape(bsz, nc, ng, rep)

    def step(h, inp):
        st, dec = inp
        return dec[..., None, None] * h + st, h

    h0 = jnp.zeros((bsz, ng, rep, hp, ns), jnp.float32)
    _, prev = lax.scan(step, h0, (jnp.moveaxis(states.astype(jnp.float32), 1, 0),
                                  jnp.moveaxis(chunk_decay, 1, 0)))
    prev = jnp.moveaxis(prev, 0, 1)
    y_off = jnp.einsum('bclgn,bcgrpn,bclgr->bclgrp', cc, prev,
                       jnp.exp(a_cs).reshape(bsz, nc, q, ng, rep))
    return (y_diag + y_off).reshape(bsz, seq, nh, hp)


def ssd_mixer(z, xbc, dt_raw, conv_w, conv_b, dt_bias, a_log, d_skip, norm_w):
    bsz, seq = z.shape[:2]
    xbc = jax.nn.silu(causal_dwconv(xbc, conv_w, conv_b))
    xs, bm, cm = jnp.split(xbc, [SSD_WIDTH, SSD_WIDTH + SSD_GROUPS * SSD_STATE], axis=-1)
    xs = xs.reshape(bsz, seq, SSD_HEADS, SSD_HEAD_DIM)
    bm = bm.reshape(bsz, seq, SSD_GROUPS, SSD_STATE)
    cm = cm.reshape(bsz, seq, SSD_GROUPS, SSD_STATE)
    dt = jax.nn.softplus(dt_raw.astype(jnp.float32) + dt_bias.astype(jnp.float32))
    a_head = -jnp.exp(a_log.astype(jnp.float32))
    y = ssd_scan(xs, dt, a_head, bm, cm) + d_skip[:, None] * xs
    y = y.reshape(bsz, seq, SSD_WIDTH) * jax.nn.silu(z)
    yg = y.astype(jnp.float32).reshape(bsz, seq, SSD_GROUPS, SSD_WIDTH // SSD_GROUPS)
    yg = yg * lax.rsqrt(jnp.mean(yg * yg, axis=-1, keepdims=True) + RMS_EPS)
    return (yg.reshape(bsz, seq, SSD_WIDTH) * norm_w).astype(z.dtype)


def even_mixer(x, w_in, conv_w, conv_b, dt_bias, a_log, d_skip, ssd_norm, w_out):
    bsz, seq = x.shape[:2]
    proj = x @ w_in
    cuts = [A_WIDTH, 2 * A_WIDTH, 3 * A_WIDTH, 3 * A_WIDTH + SSD_WIDTH,
            3 * A_WIDTH + SSD_WIDTH + SSD_CONV_WIDTH]
    q, k, v, z, xbc, dt_raw = jnp.split(proj, cuts, axis=-1)
    heads = lambda a: a.reshape(bsz, seq, A_HEADS, HEAD_DIM)
    y_a = dilated_attention(heads(q), heads(k), heads(v))
    y_b = ssd_mixer(z, xbc, dt_raw, conv_w, conv_b, dt_bias, a_log, d_skip, ssd_norm)
    return jnp.concatenate([y_a, y_b.astype(y_a.dtype)], axis=-1) @ w_out


def moba_attention(q, k, v):
    bsz, seq, nh, hd = q.shape
    nb = -(-seq // MOBA_BLOCK)
    sp = nb * MOBA_BLOCK
    pad = sp - seq
    scale = hd ** -0.5

    def blocks(a):
        a = jnp.pad(a, ((0, 0), (0, pad), (0, 0), (0, 0)))
        return a.reshape(bsz, nb, MOBA_BLOCK, nh, hd).transpose(0, 3, 1, 2, 4)

    qb, kb, vb = blocks(q), blocks(k), blocks(v)
    causal = jnp.tril(jnp.ones((MOBA_BLOCK, MOBA_BLOCK), dtype=bool))
    s = jnp.einsum('bhnqe,bhnke->bhnqk', qb, kb).astype(jnp.float32) * scale
    p, lse_self = softmax_stats(jnp.where(causal, s, NEG))
    o_self = jnp.einsum('bhnqk,bhnke->bhnqe', p.astype(v.dtype), vb).reshape(bsz, nh, sp, hd)
    lse_self = lse_self.reshape(bsz, nh, sp)
    qf = qb.reshape(bsz, nh, sp, hd)
    k_mean = jnp.mean(kb.astype(jnp.float32), axis=3)
    gate = jnp.einsum('bhse,bhne->bhsn', qf.astype(jnp.float32), k_mean)
    q_blk = jnp.arange(sp) // MOBA_BLOCK
    past = jnp.arange(nb)[None, :] < q_blk[:, None]
    gate = jnp.where(past, gate, NEG)
    n_sel = min(MOBA_TOPK, nb)
    _, sel = lax.top_k(gate, n_sel)
    sel_valid = jnp.arange(n_sel)[None, :] < q_blk[:, None]
    nq = sp // MOBA_QUERY_CHUNK
    qc_all = jnp.moveaxis(qf.reshape(bsz, nh, nq, MOBA_QUERY_CHUNK, hd), 2, 0)
    ic_all = jnp.moveaxis(sel.reshape(bsz, nh, nq, MOBA_QUERY_CHUNK, n_sel), 2, 0)
    vc_all = sel_valid.reshape(nq, MOBA_QUERY_CHUNK, n_sel)
    gather_blocks = jax.vmap(jax.vmap(lambda blk, idx: blk[idx]))

    def past_part(args):
        qc, ic, vc = args
        kg = gather_blocks(kb, ic)
        vg = gather_blocks(vb, ic)
        s = jnp.einsum('bhqe,bhqrle->bhqrl', qc, kg).astype(jnp.float32) * scale
        s = jnp.where(vc[None, None, :, :, None], s, NEG)
        s = s.reshape(bsz, nh, MOBA_QUERY_CHUNK, n_sel * MOBA_BLOCK)
        p, lse = softmax_stats(s)
        vg = vg.reshape(bsz, nh, MOBA_QUERY_CHUNK, n_sel * MOBA_BLOCK, hd)
        return jnp.einsum('bhqj,bhqje->bhqe', p.astype(v.dtype), vg), lse

    o_past, lse_past = lax.map(past_part, (qc_all, ic_all, vc_all))
    o_past = jnp.moveaxis(o_past, 0, 2).reshape(bsz, nh, sp, hd)
    lse_past = jnp.moveaxis(lse_past, 0, 2).reshape(bsz, nh, sp)
    w_self = jax.nn.sigmoid(lse_self - lse_past)[..., None]
    o = w_self * o_self.astype(jnp.float32) + (1.0 - w_self) * o_past.astype(jnp.float32)
    o = o[:, :, :seq].transpose(0, 2, 1, 3).reshape(bsz, seq, nh * hd)
    return o.astype(q.dtype)


def odd_mixer(x, w_qkv, w_out):
    bsz, seq = x.shape[:2]
    q, k, v = jnp.split(x @ w_qkv, 3, axis=-1)
    heads = lambda a: a.reshape(bsz, seq, C_HEADS, HEAD_DIM)
    return moba_attention(heads(q), heads(k), heads(v)) @ w_out


def swiglu(x, w_gu, w_down):
    g, u = jnp.split(x @ w_gu, 2, axis=-1)
    return (jax.nn.silu(g) * u) @ w_down


def moe_swiglu(x, w_router, b_router, w_gu, w_down):
    bsz, seq, d = x.shape
    xt = x.reshape(bsz * seq, d)
    logits = xt.astype(jnp.float32) @ w_router.astype(jnp.float32) + b_router.astype(jnp.float32)
    top_val, top_idx = lax.top_k(logits, TOP_K)
    gates = jax.nn.softmax(top_val, axis=-1)
    gate_full = jnp.sum(jax.nn.one_hot(top_idx, N_EXPERTS, dtype=jnp.float32) * gates[..., None], axis=1)
    gate_full = gate_full.astype(x.dtype)
    out = gate_full[:, 0:1] * swiglu(xt, w_gu[0], w_down[0])
    for e in range(1, N_EXPERTS):
        out = out + gate_full[:, e:e + 1] * swiglu(xt, w_gu[e], w_down[e])
    return out.reshape(bsz, seq, d)


def setup_inputs(seed: int = 0) -> dict:
    key = jax.random.key(seed)
    ks = jax.random.split(key, 32)
    f32 = jnp.float32
    nrm = lambda k, shape, sc: jax.random.normal(k, shape, f32) * sc
    x = nrm(ks[0], (BATCH, SEQ, D_MODEL), 1.0)
    ev_w_in = nrm(ks[1], (N_EVEN, D_MODEL, EVEN_IN_WIDTH), D_MODEL ** -0.5)
    ev_conv_w = nrm(ks[2], (N_EVEN, SSD_CONV, SSD_CONV_WIDTH), SSD_CONV ** -0.5)
    ev_conv_b = nrm(ks[3], (N_EVEN, SSD_CONV_WIDTH), 0.02)
    dt0 = jnp.exp(jax.random.uniform(ks[4], (N_EVEN, SSD_HEADS), f32, math.log(1e-3), math.log(1e-1)))
    ev_dt_bias = dt0 + jnp.log(-jnp.expm1(-dt0))
    ev_a_log = jnp.log(jax.random.uniform(ks[5], (N_EVEN, SSD_HEADS), f32, 1.0, 16.0))
    ev_d_skip = 1.0 + nrm(ks[6], (N_EVEN, SSD_HEADS), 0.1)
    ev_ssd_norm = 1.0 + nrm(ks[7], (N_EVEN, SSD_WIDTH), 0.02)
    ev_w_out = nrm(ks[8], (N_EVEN, MIX_WIDTH, D_MODEL), MIX_WIDTH ** -0.5 * DEEPNORM_BETA)
    ev_ln1_g = 1.0 + nrm(ks[9], (N_EVEN, D_MODEL), 0.02)
    ev_ln1_b = nrm(ks[10], (N_EVEN, D_MODEL), 0.02)
    ev_ffn_w_gu = nrm(ks[11], (N_EVEN, D_MODEL, 2 * FFN_DIM), D_MODEL ** -0.5)
    ev_ffn_w_down = nrm(ks[12], (N_EVEN, FFN_DIM, D_MODEL), FFN_DIM ** -0.5 * DEEPNORM_BETA)
    ev_ln2_g = 1.0 + nrm(ks[13], (N_EVEN, D_MODEL), 0.02)
    ev_ln2_b = nrm(ks[14], (N_EVEN, D_MODEL), 0.02)
    od_w_qkv = nrm(ks[15], (N_ODD, D_MODEL, 3 * C_WIDTH), D_MODEL ** -0.5)
    od_w_out = nrm(ks[16], (N_ODD, C_WIDTH, D_MODEL), C_WIDTH ** -0.5 * DEEPNORM_BETA)
    od_ln1_g = 1.0 + nrm(ks[17], (N_ODD, D_MODEL), 0.02)
    od_ln1_b = nrm(ks[18], (N_ODD, D_MODEL), 0.02)
    od_router_w = nrm(ks[19], (N_ODD, D_MODEL, N_EXPERTS), D_MODEL ** -0.5)
    od_router_b = nrm(ks[20], (N_ODD, N_EXPERTS), 0.01)
    od_exp_w_gu = nrm(ks[21], (N_ODD, N_EXPERTS, D_MODEL, 2 * FFN_DIM), D_MODEL ** -0.5)
    od_exp_w_down = nrm(ks[22], (N_ODD, N_EXPERTS, FFN_DIM, D_MODEL), FFN_DIM ** -0.5 * DEEPNORM_BETA)
    od_ln2_g = 1.0 + nrm(ks[23], (N_ODD, D_MODEL), 0.02)
    od_ln2_b = nrm(ks[24], (N_ODD, D_MODEL), 0.02)
    return {'x': x,
            'ev_w_in': ev_w_in, 'ev_conv_w': ev_conv_w, 'ev_conv_b': ev_conv_b,
            'ev_dt_bias': ev_dt_bias, 'ev_a_log': ev_a_log, 'ev_d_skip': ev_d_skip,
            'ev_ssd_norm': ev_ssd_norm, 'ev_w_out': ev_w_out,
            'ev_ln1_g': ev_ln1_g, 'ev_ln1_b': ev_ln1_b,
            'ev_ffn_w_gu': ev_ffn_w_gu, 'ev_ffn_w_down': ev_ffn_w_down,
            'ev_ln2_g': ev_ln2_g, 'ev_ln2_b': ev_ln2_b,
            'od_w_qkv': od_w_qkv, 'od_w_out': od_w_out,
            'od_ln1_g': od_ln1_g, 'od_ln1_b': od_ln1_b,
            'od_router_w': od_router_w, 'od_router_b': od_router_b,
            'od_exp_w_gu': od_exp_w_gu, 'od_exp_w_down': od_exp_w_down,
            'od_ln2_g': od_ln2_g, 'od_ln2_b': od_ln2_b}


def reference(x, ev_w_in, ev_conv_w, ev_conv_b, ev_dt_bias, ev_a_log, ev_d_skip,
              ev_ssd_norm, ev_w_out, ev_ln1_g, ev_ln1_b, ev_ffn_w_gu, ev_ffn_w_down,
              ev_ln2_g, ev_ln2_b, od_w_qkv, od_w_out, od_ln1_g, od_ln1_b,
              od_router_w, od_router_b, od_exp_w_gu, od_exp_w_down, od_ln2_g, od_ln2_b):
    for layer in range(DEPTH):
        i = layer // 2
        if layer % 2 == 0:
            h = even_mixer(x, ev_w_in[i], ev_conv_w[i], ev_conv_b[i], ev_dt_bias[i],
                           ev_a_log[i], ev_d_skip[i], ev_ssd_norm[i], ev_w_out[i])
            x = layer_norm(DEEPNORM_ALPHA * x + h, ev_ln1_g[i], ev_ln1_b[i])
            h = swiglu(x, ev_ffn_w_gu[i], ev_ffn_w_down[i])
            x = layer_norm(DEEPNORM_ALPHA * x + h, ev_ln2_g[i], ev_ln2_b[i])
        else:
            h = odd_mixer(x, od_w_qkv[i], od_w_out[i])
            x = layer_norm(DEEPNORM_ALPHA * x + h, od_ln1_g[i], od_ln1_b[i])
            h = moe_swiglu(x, od_router_w[i], od_router_b[i], od_exp_w_gu[i], od_exp_w_down[i])
            x = layer_norm(DEEPNORM_ALPHA * x + h, od_ln2_g[i], od_ln2_b[i])
    return x
```

```python
import os
import numpy as np
import concourse.bass as bass
import concourse.mybir as mybir
from concourse.bass_utils import run_bass_kernel_spmd
from contextlib import ExitStack

F32 = mybir.dt.float32
BF16 = mybir.dt.bfloat16
I32 = mybir.dt.int32
AF = mybir.ActivationFunctionType
ALU = mybir.AluOpType
AX = mybir.AxisListType

D = 2048
S = 2048
P = 128
DC = 16
FF = 5632
FC = 44
NE = 8
DEPTH = 4
ALPHA = float((2 * DEPTH) ** 0.25)
LN_EPS = 1e-5
RMS_EPS = 1e-5
EVEN_IN = 6160
NCORES = 8
CAP = 640
NST = CAP // 128


class Reg:
    __slots__ = ("w", "r", "name", "excl")

    def __init__(self, name="", excl=False):
        self.w = None
        self.r = []
        self.name = name
        self.excl = excl


class DSlot:
    __slots__ = ("sem", "count", "last")

    def __init__(self, sem):
        self.sem = sem
        self.count = 0
        self.last = None


class KB:
    def __init__(self, nc, es, n_dsem=20):
        self.nc = nc
        self.es = es
        self.engs = dict(pe=nc.tensor, act=nc.scalar, dve=nc.vector, pool=nc.gpsimd, sp=nc.sync)
        self.sem = {e: es.enter_context(nc.semaphore("s_" + e)) for e in ["pe", "act", "dve", "pool"]}
        self.cnt = {e: 0 for e in self.sem}
        self.waited = {e: {} for e in self.engs}
        self.dslots = {q: [DSlot(es.enter_context(nc.semaphore("d_%s%d" % (q, i)))) for i in range(n_dsem)]
                       for q in ["sp", "pool"]}
        self.dnext = {"sp": 0, "pool": 0}
        self.nbank = 0
        self.bank_pool = list(range(8))
        self.banks = []
        self.pbig = [es.enter_context(nc.psum_tensor("pbig%d" % i, [P, 2048], F32)) for i in range(2)]
        for i in range(8):
            t = self.pbig[i // 4][:, (i % 4) * 512:(i % 4 + 1) * 512]
            self.banks.append((t, Reg("bank%d" % i, excl=True)))

    def bank(self):
        pool = self.bank_pool
        b = self.banks[pool[self.nbank % len(pool)]]
        self.nbank += 1
        return b

    def _wait(self, eng, tok):
        sem, val = tok
        k = id(sem)
        if self.waited[eng].get(k, 0) < val:
            self.engs[eng].wait_ge(sem, val)
            self.waited[eng][k] = val

    def _deps(self, eng, reads, writes):
        deps = []
        for r in reads:
            if r.w is not None:
                deps.append(r.w)
        for w in writes:
            if w.w is not None:
                deps.append(w.w)
            deps.extend(w.r)
        for t in deps:
            self._wait(eng, t)

    def _commit(self, tok, reads, writes):
        for r in reads:
            r.r.append(tok)
            if len(r.r) > 64:
                best = {}
                for (s, v) in r.r:
                    if id(s) not in best or best[id(s)][1] < v:
                        best[id(s)] = (s, v)
                r.r = list(best.values())
        for w in writes:
            w.w = tok
            w.r = []

    def op(self, eng, fn, reads=(), writes=()):
        ex = [r for r in reads if r.excl]
        if ex:
            reads = [r for r in reads if not r.excl]
            writes = list(writes) + ex
        self._deps(eng, reads, writes)
        ins = fn(self.engs[eng])
        self.cnt[eng] += 1
        ins.then_inc(self.sem[eng], 1)
        tok = (self.sem[eng], self.cnt[eng])
        self._commit(tok, reads, writes)
        return tok

    def dma(self, q, out, in_, reads=(), writes=(), **kw):
        slots = self.dslots[q]
        sl = slots[self.dnext[q] % len(slots)]
        self.dnext[q] += 1
        if sl.last is not None:
            self._wait(q, sl.last)
        self._deps(q, reads, writes)
        ins = self.engs[q].dma_start(out=out, in_=in_, **kw)
        sl.count += 16
        ins.then_inc(sl.sem, 16)
        tok = (sl.sem, sl.count)
        sl.last = tok
        self._commit(tok, reads, writes)
        return tok

    def barrier(self):
        toks = [(self.sem[e], self.cnt[e]) for e in self.sem if self.cnt[e] > 0]
        for q in self.dslots:
            for sl in self.dslots[q]:
                if sl.last is not None:
                    toks.append(sl.last)
        for e in self.engs:
            for t in toks:
                self._wait(e, t)


def r3(W, p=P):
    return W.rearrange("(k p) c -> p k c", p=p)


class Ctx:
    pass


_UNIQ = [0]


def sbt(nc, es, name, shape, dt):
    _UNIQ[0] += 1
    return es.enter_context(nc.sbuf_tensor("%s_%d" % (name, _UNIQ[0]), list(shape), dt))


def stream_gemm_fm(kb, es_outer, W, KC, col_chunks, rhs_fn, rhs_regs, NTB, evac, slab=1, name="w", extra_cols=None):
    nc = kb.nc
    with ExitStack() as es:
        wb = [sbt(nc, es, "%s_wb%d" % (name, i), [P, KC, slab * 128], BF16) for i in range(2)]
        wr = [Reg(), Reg()]
        W3 = r3(W)
        ngroups = (len(col_chunks) + slab - 1) // slab
        for g in range(ngroups):
            chunks = col_chunks[g * slab:(g + 1) * slab]
            c0 = chunks[0]
            n = len(chunks)
            b = g % 2
            kb.dma("pool", wb[b][:, :, 0:n * 128], W3[:, :, c0:c0 + n * 128], writes=[wr[b]])
            for j, cc in enumerate(chunks):
                ci = g * slab + j
                for tb in range(NTB):
                    bt, br = kb.bank()
                    for kc in range(KC):
                        kb.op("pe", lambda e, kc=kc, j=j, tb=tb: e.matmul(
                            bt, lhsT=wb[b][:, kc, j * 128:(j + 1) * 128], rhs=rhs_fn(kc, tb),
                            start=(kc == 0), stop=(kc == KC - 1)),
                            reads=[wr[b]] + list(rhs_regs), writes=[br])
                    evac(ci, tb, bt, br)
        kb.barrier()


def ln_phase(kb, R, XF, gb, li, consts):
    nc = kb.nc
    with ExitStack() as es:
        rbuf = [sbt(nc, es, "ln_r%d" % i, [P, S], F32) for i in range(2)]
        rreg = [Reg(), Reg()]
        sq = [sbt(nc, es, "ln_sq%d" % i, [P, S], F32) for i in range(2)]
        sqreg = [Reg(), Reg()]
        mean = sbt(nc, es, "ln_mean", [P, S], F32)
        rstd = sbt(nc, es, "ln_rstd", [P, S], F32)
        msq = sbt(nc, es, "ln_msq", [P, S], F32)
        streg = Reg()
        banks = [kb.bank() for _ in range(8)]
        for dc in range(DC):
            b = dc % 2
            kb.dma("sp", rbuf[b][:], R[dc], writes=[rreg[b]])
            kb.op("act", lambda e: e.activation(out=sq[b][:], in_=rbuf[b][:], func=AF.Square),
                  reads=[rreg[b]], writes=[sqreg[b]])
            for tb in range(4):
                kb.op("pe", lambda e: e.matmul(banks[tb][0], lhsT=consts.ones_f[:], rhs=rbuf[b][:, tb * 512:(tb + 1) * 512],
                                               start=(dc == 0), stop=(dc == DC - 1)),
                      reads=[rreg[b]], writes=[banks[tb][1]])
                kb.op("pe", lambda e: e.matmul(banks[4 + tb][0], lhsT=consts.ones_f[:], rhs=sq[b][:, tb * 512:(tb + 1) * 512],
                                               start=(dc == 0), stop=(dc == DC - 1)),
                      reads=[sqreg[b]], writes=[banks[4 + tb][1]])
        for tb in range(4):
            sl = slice(tb * 512, (tb + 1) * 512)
            kb.op("dve", lambda e: e.tensor_scalar(out=mean[:, sl], in0=banks[tb][0], scalar1=1.0 / D, scalar2=None,
                                                   op0=ALU.mult), reads=[banks[tb][1]], writes=[streg])
            kb.op("dve", lambda e: e.tensor_tensor(out=msq[:, sl], in0=mean[:, sl], in1=mean[:, sl], op=ALU.mult),
                  reads=[streg], writes=[streg])
            kb.op("dve", lambda e: e.scalar_tensor_tensor(out=rstd[:, sl], in0=banks[4 + tb][0], scalar=1.0 / D,
                                                          in1=msq[:, sl], op0=ALU.mult, op1=ALU.subtract),
                  reads=[banks[4 + tb][1], streg], writes=[streg])
            kb.op("dve", lambda e: e.tensor_scalar(out=rstd[:, sl], in0=rstd[:, sl], scalar1=LN_EPS, scalar2=None,
                                                   op0=ALU.add), reads=[streg], writes=[streg])
            kb.op("act", lambda e: e.activation(out=rstd[:, sl], in_=rstd[:, sl], func=AF.Sqrt),
                  reads=[streg], writes=[streg])
            kb.op("dve", lambda e: e.reciprocal(out=rstd[:, sl], in_=rstd[:, sl]), reads=[streg], writes=[streg])
        for dc in range(DC):
            b = dc % 2
            kb.dma("sp", rbuf[b][:], R[dc], writes=[rreg[b]])
            kb.op("dve", lambda e: e.tensor_tensor(out=sq[b][:], in0=rbuf[b][:], in1=mean[:], op=ALU.subtract),
                  reads=[rreg[b], streg], writes=[sqreg[b]])
            kb.op("pool", lambda e: e.tensor_tensor(out=sq[b][:], in0=sq[b][:], in1=rstd[:], op=ALU.mult),
                  reads=[sqreg[b], streg], writes=[sqreg[b]])
            kb.op("act", lambda e: e.activation(out=sq[b][:], in_=sq[b][:], func=AF.Identity,
                                                scale=gb[:, li, 0, dc:dc + 1], bias=gb[:, li, 1, dc:dc + 1]),
                  reads=[sqreg[b]], writes=[sqreg[b]])
            kb.dma("sp", XF[dc], sq[b][:], reads=[sqreg[b]])
        kb.barrier()


def ffn_dense(kb, XF, R, Wgu, Wdown, consts, gate_dram=None, acc=False):
    nc = kb.nc
    TB = 1024
    for pss in range(S // TB):
        t0 = pss * TB
        with ExitStack() as es:
            xb = sbt(nc, es, "ffn_xb", [P, DC, TB], BF16)
            xreg = Reg()
            at = sbt(nc, es, "ffn_at", [P, FC, TB], BF16)
            areg = [Reg() for _ in range(FC)]
            sg = [sbt(nc, es, "ffn_sg%d" % i, [P, 512], F32) for i in range(2)]
            sgreg = [Reg(), Reg()]
            for dc in range(DC):
                kb.dma("pool", xb[:, dc, :], XF[dc][:, t0:t0 + TB], writes=[xreg])
            gate = None
            if gate_dram is not None:
                gt = sbt(nc, es, "ffn_gate", [P, TB], F32)
                greg = Reg()
                kb.dma("sp", gt[:], gate_dram[t0:t0 + TB].partition_broadcast(P), writes=[greg])
                gate = (gt, greg)
            state = {"n": 0}

            def evac_up(ci, tb, bt, br):
                if ci % 2 == 0:
                    i = state["n"] % 2
                    state["n"] += 1
                    kb.op("act", lambda e: e.activation(out=sg[i][:], in_=bt, func=AF.Silu),
                          reads=[br], writes=[sgreg[i]])
                    state[("g", tb)] = i
                else:
                    i = state[("g", tb)]
                    fc = ci // 2
                    kb.op("dve", lambda e: e.tensor_tensor(out=at[:, fc, tb * 512:(tb + 1) * 512], in0=sg[i][:], in1=bt,
                                                           op=ALU.mult), reads=[br, sgreg[i]], writes=[areg[fc]])

            chunks = []
            for fc in range(FC):
                chunks += [fc * 128, FF + fc * 128]
            stream_gemm_fm(kb, es, Wgu, DC, chunks, lambda kc, tb: xb[:, kc, tb * 512:(tb + 1) * 512], [xreg],
                           TB // 512, evac_up, slab=1, name="gu")
            evac_down = resid_evac_factory(kb, es, R if acc else XF, R, t0, TB, "ffn", alpha=(1.0 if acc else ALPHA), gate=gate)
            stream_gemm_fm(kb, es, Wdown, FC, [dc * 128 for dc in range(DC)],
                           lambda kc, tb: at[:, kc, tb * 512:(tb + 1) * 512], areg, TB // 512, evac_down, slab=1, name="dn")
        kb.barrier()


def moe_router(kb, XF, Gd, wr_sb, br_sb, consts):
    nc = kb.nc
    with ExitStack() as es:
        xt = [sbt(nc, es, "rt_x%d" % i, [P, DC, P], F32) for i in range(2)]
        xtreg = [Reg(), Reg()]
        lg = sbt(nc, es, "rt_lg", [P, 16, 8], F32)
        t8 = sbt(nc, es, "rt_t8", [P, 16, 8], F32)
        g1 = sbt(nc, es, "rt_g1", [P, 16, 2], F32)
        m1 = sbt(nc, es, "rt_m1", [P, 16, 8], F32)
        m2 = sbt(nc, es, "rt_m2", [P, 16, 8], F32)
        lreg = Reg()
        for tt in range(16):
            b = tt % 2
            with nc.allow_non_contiguous_dma(reason="router token tile"):
                kb.dma("sp", xt[b][:], XF[:, :, tt * 128:(tt + 1) * 128].rearrange("c p t -> p c t"), writes=[xtreg[b]])
            bt, br = kb.bank()
            for dc in range(DC):
                kb.op("pe", lambda e: e.matmul(bt[:, 0:8], lhsT=xt[b][:, dc, :], rhs=wr_sb[:, dc, :], start=(dc == 0),
                                               stop=(dc == DC - 1)), reads=[xtreg[b], consts.reg], writes=[br])
            kb.op("dve", lambda e: e.tensor_tensor(out=lg[:, tt, :], in0=bt[:, 0:8], in1=br_sb[:], op=ALU.add),
                  reads=[br, consts.reg], writes=[lreg])
            kb.op("dve", lambda e: e.max(out=t8[:, tt, :], in_=lg[:, tt, :]), reads=[lreg], writes=[lreg])
            kb.op("dve", lambda e: e.tensor_tensor(out=g1[:, tt, 0:1], in0=t8[:, tt, 0:1], in1=t8[:, tt, 1:2], op=ALU.subtract),
                  reads=[lreg], writes=[lreg])
            kb.op("act", lambda e: e.activation(out=g1[:, tt, 0:1], in_=g1[:, tt, 0:1], func=AF.Sigmoid), reads=[lreg], writes=[lreg])
            kb.op("dve", lambda e: e.tensor_scalar(out=g1[:, tt, 1:2], in0=g1[:, tt, 0:1], scalar1=-1.0, scalar2=1.0,
                                                   op0=ALU.mult, op1=ALU.add), reads=[lreg], writes=[lreg])
            kb.op("dve", lambda e: e.tensor_scalar(out=m1[:, tt, :], in0=lg[:, tt, :], scalar1=t8[:, tt, 0:1], scalar2=g1[:, tt, 0:1],
                                                   op0=ALU.is_equal, op1=ALU.mult), reads=[lreg], writes=[lreg])
            kb.op("dve", lambda e: e.tensor_scalar(out=m2[:, tt, :], in0=lg[:, tt, :], scalar1=t8[:, tt, 1:2], scalar2=g1[:, tt, 1:2],
                                                   op0=ALU.is_equal, op1=ALU.mult), reads=[lreg], writes=[lreg])
            kb.op("dve", lambda e: e.tensor_tensor(out=m1[:, tt, :], in0=m1[:, tt, :], in1=m2[:, tt, :], op=ALU.add),
                  reads=[lreg], writes=[lreg])
            with nc.allow_non_contiguous_dma(reason="gate transpose store"):
                kb.dma("sp", Gd[:, tt * 128:(tt + 1) * 128].rearrange("e t -> t e"), m1[:, tt, :], reads=[lreg])
        kb.barrier()


def moe_dense(kb, XF, R, Gd, Wgu8, Wdown8, wr_sb, br_sb, consts):
    moe_router(kb, XF, Gd, wr_sb, br_sb, consts)
    for e_ in range(NE):
        ffn_dense(kb, XF, R, Wgu8[e_], Wdown8[e_], consts, gate_dram=Gd[e_], acc=(e_ > 0))


SCALE = float(128 ** -0.5)


def transposes_bf16(kb, consts, srcs, src_regs, dst3, dst_regs, eng):
    bt, br = kb.bank()
    bb = bt.bitcast(BF16)
    n = len(srcs)
    for j, sap in enumerate(srcs):
        kb.op("pe", lambda e: e.transpose(out=bb[:, j * 128:(j + 1) * 128], in_=sap, identity=consts.ident_b[:]),
              reads=list(src_regs) + [consts.reg], writes=[br])
    src3 = bb[:, 0:n * 128].rearrange("p (a b) -> p a b", b=128)
    if eng == "act":
        kb.op("act", lambda e: e.activation(out=dst3, in_=src3, func=AF.Copy), reads=[br], writes=list(dst_regs))
    else:
        kb.op("dve", lambda e: e.tensor_copy(out=dst3, in_=src3), reads=[br], writes=list(dst_regs))


def attn_heads(kb, XB, xreg, YB, yreg, W, cols_fn, nheads, mode, consts, ych0):
    nc = kb.nc
    moba = mode == "moba"
    with ExitStack() as es:
        qb = sbt(nc, es, "qb", [P, S], BF16)
        kbf = sbt(nc, es, "kbf", [P, S], BF16)
        vT = sbt(nc, es, "vT", [P, S], BF16)
        qreg, kreg, vreg = Reg(), Reg(), Reg()
        vtok = sbt(nc, es, "vtok", [P, 16, 132], BF16)
        vtreg = Reg()
        pbuf = [sbt(nc, es, "pb%d" % i, [P, S], BF16) for i in range(2)]
        pbreg = [Reg(), Reg()]
        pT = [sbt(nc, es, "pT%d" % i, [P, 16, P], BF16) for i in range(2)]
        pTreg = [Reg(), Reg()]
        on = [sbt(nc, es, "on%d" % i, [P, P], BF16) for i in range(2)]
        onreg = [Reg(), Reg()]
        mx = sbt(nc, es, "mx", [P, 8], F32)
        mxreg = Reg()
        if moba:
            qf = sbt(nc, es, "qf", [P, S], F32)
            qfreg = Reg()
            ksum = sbt(nc, es, "ksum", [P, 8], F32)
            ksreg = Reg()
            gm = sbt(nc, es, "gm", [P, 16, 8], F32)
            top8 = sbt(nc, es, "top8", [P, 16, 8], F32)
            selb = sbt(nc, es, "selb", [P, 16, 8], F32)
            bj = sbt(nc, es, "bj", [P, 2, 8], F32)
            gmreg = Reg()
            bjreg = [Reg(), Reg()]
        kb.op("dve", lambda e: e.memset(vtok[:, :, 128:129], 1.0), writes=[vtreg])
        sbanks = [kb.banks[i] for i in range(4)]
        sregs = [b[1] for b in sbanks]
        Sbig = kb.pbig[0]
        kb.bank_pool = [4, 5, 6, 7]
        cnt = 0
        for h in range(int(os.environ.get('ATT_HEADS', nheads))):
            qc, kc_, vc = cols_fn(h)

            def evac(ci, tb, bt, br):
                sl = slice(tb * 512, (tb + 1) * 512)
                if ci == 0:
                    kb.op("act", lambda e: e.activation(out=qb[:, sl], in_=bt, func=AF.Copy), reads=[br], writes=[qreg])
                    if moba:
                        kb.op("dve", lambda e: e.tensor_copy(out=qf[:, sl], in_=bt), reads=[br], writes=[qfreg])
                elif ci == 1:
                    kb.op("act", lambda e: e.activation(out=kbf[:, sl], in_=bt, func=AF.Copy), reads=[br], writes=[kreg])
                    if moba:
                        kb.op("dve", lambda e: e.reduce_sum(out=ksum[:, 2 * tb:2 * tb + 2],
                                                            in_=bt.rearrange("p (b k) -> p b k", k=256), axis=AX.X),
                              reads=[br], writes=[ksreg])
                else:
                    kb.op("dve", lambda e: e.tensor_copy(out=vT[:, sl], in_=bt), reads=[br], writes=[vreg])

            stream_gemm_fm(kb, es, W, DC, [qc, kc_, vc], lambda kc, tb: XB[:, kc, tb * 512:(tb + 1) * 512], [xreg], 4,
                           evac, slab=1, name="qkv")
            if os.environ.get("ATT_STOP") == "1":
                continue
            for t4 in range(4):
                transposes_bf16(kb, consts, [vT[:, (t4 * 4 + j) * 128:(t4 * 4 + j + 1) * 128] for j in range(4)], [vreg],
                                vtok[:, t4 * 4:t4 * 4 + 4, 0:128], [vtreg], "act" if t4 % 2 else "dve")
            if os.environ.get("ATT_STOP") == "2":
                continue
            if moba:
                kb.op("dve", lambda e: e.tensor_scalar(out=ksum[:], in0=ksum[:], scalar1=1.0 / 256, scalar2=None, op0=ALU.mult),
                      reads=[ksreg], writes=[ksreg])
                gt, gr = kb.bank()
                for qt in range(16):
                    kb.op("pe", lambda e: e.matmul(gt[:, qt * 8:(qt + 1) * 8], lhsT=qf[:, qt * 128:(qt + 1) * 128], rhs=ksum[:, 0:8],
                                                   start=True, stop=True), reads=[qfreg, ksreg], writes=[gr])
                kb.op("dve", lambda e: e.tensor_tensor(out=gm[:].rearrange("p a b -> p (a b)"), in0=gt[:, 0:128],
                                                       in1=consts.negmask[:], op=ALU.add), reads=[gr, consts.reg], writes=[gmreg])
                for qt in range(16):
                    kb.op("dve", lambda e: e.max(out=top8[:, qt, :], in_=gm[:, qt, :]), reads=[gmreg], writes=[gmreg])
                for qt in range(16):
                    kb.op("dve", lambda e: e.tensor_scalar(out=selb[:, qt, :], in0=gm[:, qt, :], scalar1=top8[:, qt, 2:3],
                                                           scalar2=None, op0=ALU.is_ge), reads=[gmreg], writes=[gmreg])
                kb.op("dve", lambda e: e.tensor_scalar(out=selb[:].rearrange("p a b -> p (a b)"),
                                                       in0=selb[:].rearrange("p a b -> p (a b)"), scalar1=-1.0, scalar2=30000.0,
                                                       op0=ALU.add, op1=ALU.mult), reads=[gmreg], writes=[gmreg])
            for qt in range(int(os.environ.get('ATT_QT', 16))):
                i = cnt % 2
                cnt += 1
                if moba:
                    n = qt // 2
                    nk = (n + 1) * 256
                else:
                    nk = (qt + 1) * 128
                nch = (nk + 511) // 512
                for c in range(nch):
                    w = min(512, nk - c * 512)
                    kb.op("pe", lambda e: e.matmul(sbanks[c][0][:, 0:w], lhsT=qb[:, qt * 128:(qt + 1) * 128],
                                                   rhs=kbf[:, c * 512:c * 512 + w], start=True, stop=True),
                          reads=[qreg, kreg], writes=[sregs[c]])
                kb.op("dve", lambda e: e.reduce_max(out=mx[:, 0:1], in_=Sbig[:, 0:nk], axis=AX.X),
                      reads=sregs[:nch], writes=[mxreg])
                kb.op("dve", lambda e: e.tensor_scalar(out=mx[:, 1:2], in0=mx[:, 0:1], scalar1=-SCALE, scalar2=None, op0=ALU.mult),
                      reads=[mxreg], writes=[mxreg])
                pb = pbuf[i]
                if moba:
                    if n > 0:
                        kb.op("dve", lambda e: e.tensor_scalar(out=bj[:, i, :], in0=selb[:, qt, :], scalar1=mx[:, 1:2], scalar2=None,
                                                               op0=ALU.add), reads=[gmreg, mxreg], writes=[bjreg[i]])
                    for j in range(n):
                        kb.op("act", lambda e: e.activation(out=pb[:, j * 256:(j + 1) * 256], in_=Sbig[:, j * 256:(j + 1) * 256],
                                                            func=AF.Exp, scale=SCALE, bias=bj[:, i, j:j + 1]),
                              reads=sregs[:nch] + [bjreg[i]], writes=[pbreg[i]])
                    kb.op("act", lambda e: e.activation(out=pb[:, n * 256:(n + 1) * 256], in_=Sbig[:, n * 256:(n + 1) * 256],
                                                        func=AF.Exp, scale=SCALE, bias=mx[:, 1:2]),
                          reads=sregs[:nch] + [mxreg], writes=[pbreg[i]])
                    kb.op("dve", lambda e: e.tensor_tensor(out=pb[:, n * 256:(n + 1) * 256], in0=pb[:, n * 256:(n + 1) * 256],
                                                           in1=consts.causal[:, qt % 2, :], op=ALU.mult),
                          reads=[pbreg[i], consts.reg], writes=[pbreg[i]])
                else:
                    kb.op("act", lambda e: e.activation(out=pb[:, 0:nk], in_=Sbig[:, 0:nk], func=AF.Exp, scale=SCALE,
                                                        bias=mx[:, 1:2]), reads=sregs[:nch] + [mxreg], writes=[pbreg[i]])
                    off = 128 * (15 - qt)
                    kb.op("dve", lambda e: e.tensor_tensor(out=pb[:, 0:nk], in0=pb[:, 0:nk], in1=consts.gdil[:, off:off + nk],
                                                           op=ALU.mult), reads=[pbreg[i], consts.reg], writes=[pbreg[i]])
                nkt = nk // 128
                for k4 in range((nkt + 3) // 4):
                    ks = list(range(k4 * 4, min(nkt, k4 * 4 + 4)))
                    transposes_bf16(kb, consts, [pb[:, k * 128:(k + 1) * 128] for k in ks], [pbreg[i]],
                                    pT[i][:, ks[0]:ks[-1] + 1, :], [pTreg[i]], "act" if k4 % 2 else "dve")
                ot, orr = kb.bank()
                for k in range(nkt):
                    kb.op("pe", lambda e: e.matmul(ot[:, 0:129], lhsT=pT[i][:, k, :], rhs=vtok[:, k, 0:129],
                                                   start=(k == 0), stop=(k == nkt - 1)),
                          reads=[pTreg[i], vtreg], writes=[orr])
                kb.op("dve", lambda e: e.reciprocal(out=mx[:, 2 + i:3 + i], in_=ot[:, 128:129]), reads=[orr], writes=[mxreg])
                kb.op("dve", lambda e: e.tensor_scalar(out=on[i][:], in0=ot[:, 0:128], scalar1=mx[:, 2 + i:3 + i], scalar2=None,
                                                       op0=ALU.mult), reads=[orr, mxreg], writes=[onreg[i]])
                transposes_bf16(kb, consts, [on[i][:]], [onreg[i]],
                                YB[:, ych0 + h:ych0 + h + 1, qt * 128:(qt + 1) * 128], [yreg], "act")
        kb.bank_pool = list(range(8))
        kb.barrier()


def resid_evac_factory(kb, es, XFin, R, t0, TB, name, alpha=ALPHA, gate=None):
    nc = kb.nc
    xf = [sbt(nc, es, "%s_xf%d" % (name, i), [P, TB], F32) for i in range(2)]
    xfreg = [Reg(), Reg()]
    rb = [sbt(nc, es, "%s_rb%d" % (name, i), [P, 512], F32) for i in range(4)]
    rbreg = [Reg() for _ in range(4)]
    st2 = {"n": 0}

    def evac(ci, tb, bt, br):
        b = ci % 2
        if tb == 0:
            kb.dma("sp", xf[b][:], XFin[ci][:, t0:t0 + TB], writes=[xfreg[b]])
        i = st2["n"] % 4
        st2["n"] += 1
        src = bt
        rds = [br, xfreg[b]]
        if gate is not None:
            kb.op("dve", lambda e: e.tensor_tensor(out=rb[i][:], in0=bt, in1=gate[0][:, tb * 512:(tb + 1) * 512], op=ALU.mult),
                  reads=[br, gate[1]], writes=[rbreg[i]])
            src = rb[i][:]
            rds = [rbreg[i], xfreg[b]]
        kb.op("dve", lambda e: e.scalar_tensor_tensor(out=rb[i][:], in0=xf[b][:, tb * 512:(tb + 1) * 512], scalar=alpha,
                                                      in1=src, op0=ALU.mult, op1=ALU.add),
              reads=rds, writes=[rbreg[i]])
        kb.dma("sp", R[ci][:, t0 + tb * 512:t0 + (tb + 1) * 512], rb[i][:], reads=[rbreg[i]])

    return evac


def out_proj(kb, XFin, R, YB, yreg, Wout):
    with ExitStack() as es:
        evac = resid_evac_factory(kb, es, XFin, R, 0, S, "op")
        stream_gemm_fm(kb, es, Wout, DC, [dc * 128 for dc in range(DC)],
                       lambda kc, tb: YB[:, kc, tb * 512:(tb + 1) * 512], [yreg], 4, evac, slab=1, name="wo")


def load_xb(kb, XB, xreg, XF):
    for dc in range(DC):
        for hf in range(2):
            kb.dma("pool", XB[:, dc, hf * 1024:(hf + 1) * 1024], XF[dc][:, hf * 1024:(hf + 1) * 1024], writes=[xreg])


def odd_mixer(kb, XF, R, Wqkv, Wout, consts):
    nc = kb.nc
    with ExitStack() as es:
        XB = sbt(nc, es, "XB", [P, DC, S], BF16)
        YB = sbt(nc, es, "YB", [P, DC, S], BF16)
        xreg, yreg = Reg(), Reg()
        load_xb(kb, XB, xreg, XF)
        attn_heads(kb, XB, xreg, YB, yreg, Wqkv, lambda h: (h * 128, 2048 + h * 128, 4096 + h * 128), 16, "moba", consts, 0)
        out_proj(kb, XF, R, YB, yreg, Wout)
    kb.barrier()


def ssd_branch(kb, es_persist, Win, sp, li, consts, scr):
    nc = kb.nc
    es = es_persist
    xdt = sbt(nc, es, "xdt", [P, 16, 1024], BF16)
    BT = sbt(nc, es, "BT", [P, 4, S], BF16)
    CT = sbt(nc, es, "CT", [P, 4, S], BF16)
    acst = sbt(nc, es, "acst", [P, 16, 16], F32)
    xdtreg, btreg, ctreg, acreg = Reg(), Reg(), Reg(), Reg()
    xs_d, zs_d, acs_d = scr["xs_d"], scr["zs_d"], scr["acs_d"]

    def phase_a(XB, xreg):
        with ExitStack() as es2:
            phase_a_body(es2, XB, xreg)

    def phase_a_body(es2, XB, xreg):
        dtt = sbt(nc, es2, "dtt", [P, 16, 16], F32)
        dtreg = Reg()
        with ExitStack() as es3:
            wdt = sbt(nc, es3, "wdt", [P, DC, 16], BF16)
            dt = sbt(nc, es3, "dt", [16, S], F32)
            acs = sbt(nc, es3, "acs", [16, S], F32)
            one16 = sbt(nc, es3, "one16", [16, S], F32)
            ah = sbt(nc, es3, "ah", [16, 2], F32)
            r1 = Reg()
            wreg = Reg()
            with nc.allow_non_contiguous_dma(reason="dt weights"):
                kb.dma("pool", wdt[:], r3(Win)[:, :, 6144:6160], writes=[wreg])
            kb.op("pool", lambda e: e.memset(one16[:], 1.0), writes=[r1])
            kb.op("act", lambda e: e.activation(out=ah[:, 0:1], in_=sp.alog[:, li:li + 1], func=AF.Exp), reads=[consts.reg], writes=[r1])
            kb.op("dve", lambda e: e.tensor_scalar(out=ah[:, 1:2], in0=ah[:, 0:1], scalar1=-1.0, scalar2=None, op0=ALU.mult),
                  reads=[r1], writes=[r1])
            for tb in range(4):
                bt, br = kb.bank()
                for kc in range(DC):
                    kb.op("pe", lambda e: e.matmul(bt[0:16, :], lhsT=wdt[:, kc, :], rhs=XB[:, kc, tb * 512:(tb + 1) * 512],
                                                   start=(kc == 0), stop=(kc == DC - 1)), reads=[wreg, xreg], writes=[br])
                sl = slice(tb * 512, (tb + 1) * 512)
                kb.op("act", lambda e: e.activation(out=dt[:, sl], in_=bt[0:16, :], func=AF.Exp, bias=sp.dtb[:, li:li + 1]),
                      reads=[br, consts.reg], writes=[r1])
            kb.op("dve", lambda e: e.tensor_scalar(out=dt[:], in0=dt[:], scalar1=1.0, scalar2=None, op0=ALU.add), reads=[r1], writes=[r1])
            kb.op("act", lambda e: e.activation(out=dt[:], in_=dt[:], func=AF.Ln), reads=[r1], writes=[r1])
            kb.op("dve", lambda e: e.tensor_scalar(out=acs[:], in0=dt[:], scalar1=ah[:, 1:2], scalar2=None, op0=ALU.mult),
                  reads=[r1], writes=[r1])
            kb.op("dve", lambda e: e.tensor_tensor_scan(out=acs[:], data0=one16[:], data1=acs[:], initial=0.0,
                                                        op0=ALU.mult, op1=ALU.add), reads=[r1], writes=[r1])
            kb.dma("sp", acs_d, acs[:], reads=[r1])
            for src, dst, dreg in ((dt, dtt, dtreg), (acs, acst, acreg)):
                bt, br = kb.bank()
                for tt in range(16):
                    kb.op("pe", lambda e: e.transpose(out=bt[:, tt * 16:(tt + 1) * 16], in_=src[:, tt * 128:(tt + 1) * 128],
                                                      identity=consts.ident_f[0:16, 0:16]), reads=[r1, consts.reg], writes=[br])
                kb.op("dve", lambda e: e.tensor_copy(out=dst[:].rearrange("p a b -> p (a b)"), in_=bt[:, 0:256]),
                      reads=[br], writes=[dreg])
            kb.barrier()
        with ExitStack() as es3:
            raw = [sbt(nc, es3, "raw%d" % i, [P, S], F32) for i in range(2)]
            rawreg = [Reg(), Reg()]
            cv = [sbt(nc, es3, "cv%d" % i, [P, S], F32) for i in range(2)]
            cvreg = [Reg(), Reg()]
            fb = [sbt(nc, es3, "fb%d" % i, [P, S], BF16) for i in range(2)]
            fbreg = [Reg(), Reg()]
            stg = [sbt(nc, es3, "stg%d" % i, [P, 4, P], BF16) for i in range(4)]
            stgreg = [Reg() for _ in range(4)]
            cnt = {"s": 0}

            def evac(ci, tb, bt, br):
                b = ci % 2
                sl = slice(tb * 512, (tb + 1) * 512)
                if ci < 8:
                    kb.op("act", lambda e: e.activation(out=fb[b][:, sl], in_=bt, func=AF.Silu), reads=[br], writes=[fbreg[b]])
                else:
                    kb.op("act", lambda e: e.activation(out=raw[b][:, sl], in_=bt, func=AF.Copy), reads=[br], writes=[rawreg[b]])
                if tb != 3:
                    return
                if ci >= 8:
                    c = ci - 8
                    cw = lambda k: sp.cw[:, li, c, k:k + 1]
                    kb.op("dve", lambda e: e.tensor_scalar(out=cv[b][:], in0=raw[b][:], scalar1=cw(3), scalar2=None, op0=ALU.mult),
                          reads=[rawreg[b], consts.reg], writes=[cvreg[b]])
                    for sh, k in ((1, 2), (2, 1), (3, 0)):
                        eng = "dve" if sh != 2 else "pool"
                        if eng == "dve":
                            kb.op("dve", lambda e: e.scalar_tensor_tensor(out=cv[b][:, sh:], in0=raw[b][:, 0:S - sh], scalar=cw(k),
                                                                          in1=cv[b][:, sh:], op0=ALU.mult, op1=ALU.add),
                                  reads=[rawreg[b], cvreg[b]], writes=[cvreg[b]])
                        else:
                            kb.op("dve", lambda e: e.scalar_tensor_tensor(out=cv[b][:, sh:], in0=raw[b][:, 0:S - sh], scalar=cw(k),
                                                                          in1=cv[b][:, sh:], op0=ALU.mult, op1=ALU.add),
                                  reads=[rawreg[b], cvreg[b]], writes=[cvreg[b]])
                    if c < 8:
                        dstb, dreg = fb[b][:], fbreg[b]
                    elif c < 12:
                        dstb, dreg = BT[:, c - 8, :], btreg
                    else:
                        dstb, dreg = CT[:, c - 12, :], ctreg
                    kb.op("act", lambda e: e.activation(out=dstb, in_=cv[b][:], func=AF.Silu, bias=sp.cb[:, li, c:c + 1]),
                          reads=[cvreg[b], consts.reg], writes=[dreg])
                    if c >= 8:
                        return
                isz = ci < 8
                c = ci if isz else ci - 8
                for t4 in range(4):
                    bt2, br2 = kb.bank()
                    bb = bt2.bitcast(BF16)
                    for j in range(4):
                        tt = t4 * 4 + j
                        kb.op("pe", lambda e: e.transpose(out=bb[:, j * 128:(j + 1) * 128], in_=fb[b][:, tt * 128:(tt + 1) * 128],
                                                          identity=consts.ident_b[:]), reads=[fbreg[b], consts.reg], writes=[br2])
                    i = cnt["s"] % 4
                    cnt["s"] += 1
                    src3 = bb[:, 0:512].rearrange("p (a b) -> p a b", b=128)
                    kb.op("act", lambda e: e.activation(out=stg[i][:], in_=src3, func=AF.Copy), reads=[br2], writes=[stgreg[i]])
                    if not isz:
                        for j in range(4):
                            tt = t4 * 4 + j
                            for hh in range(2):
                                kb.op("dve", lambda e: e.tensor_scalar(
                                    out=xdt[:, tt, c * 128 + hh * 64:c * 128 + (hh + 1) * 64],
                                    in0=bb[:, j * 128 + hh * 64:j * 128 + (hh + 1) * 64],
                                    scalar1=dtt[:, tt, 2 * c + hh:2 * c + hh + 1], scalar2=None, op0=ALU.mult),
                                    reads=[br2, dtreg], writes=[xdtreg])
                    dd = zs_d if isz else xs_d
                    kb.dma("sp", dd[t4 * 4:t4 * 4 + 4, :, c * 128:(c + 1) * 128].rearrange("a p f -> p a f"), stg[i][:],
                           reads=[stgreg[i]])

            chunks = [3072 + c * 128 for c in range(8)] + [4096 + c * 128 for c in range(16)]
            stream_gemm_fm(kb, es3, Win, DC, chunks, lambda kc, tb: XB[:, kc, tb * 512:(tb + 1) * 512], [xreg], 4, evac,
                           slab=1, name="xbc")
        kb.barrier()

    def phase_b(YSd):
        with ExitStack() as es4:
            rows = sbt(nc, es4, "rows", [P, 4, S], F32)
            rowreg = [Reg() for _ in range(4)]
            E = [sbt(nc, es4, "E%d" % i, [P, S], F32) for i in range(2)]
            Ereg = [Reg(), Reg()]
            dg = [sbt(nc, es4, "dg%d" % i, [P, P], F32) for i in range(2)]
            dgreg = [Reg(), Reg()]
            pbuf = [sbt(nc, es4, "sp%d" % i, [P, S], BF16) for i in range(2)]
            pbreg = [Reg(), Reg()]
            pT = [sbt(nc, es4, "spT%d" % i, [P, 16, P], BF16) for i in range(2)]
            pTreg = [Reg(), Reg()]
            YS = sbt(nc, es4, "YS", [P, 8, S], BF16)
            ysreg = Reg()
            xz = [sbt(nc, es4, "xz%d" % i, [P, 2, 256], BF16) for i in range(2)]
            xzreg = [Reg(), Reg()]
            y1 = [sbt(nc, es4, "y1_%d" % i, [P, 256], F32) for i in range(2)]
            y1reg = [Reg(), Reg()]
            yb = [sbt(nc, es4, "yb_%d" % i, [P, 256], BF16) for i in range(2)]
            ybreg = [Reg(), Reg()]
            ss = sbt(nc, es4, "ss", [P, 4], F32)
            ssreg = Reg()
            sbanks = [kb.banks[i] for i in range(4)]
            sregs = [b[1] for b in sbanks]
            Sbig = kb.pbig[0]
            kb.bank_pool = [5, 6, 7]
            cnt = 0
            cg = 0
            for g in range(4):
                for hl in range(4):
                    h = g * 4 + hl
                    for hf in range(2):
                        kb.dma("sp", rows[:, hl, hf * 1024:(hf + 1) * 1024], acs_d[h, hf * 1024:(hf + 1) * 1024].partition_broadcast(P),
                               writes=[rowreg[hl]])
                for lt in range(16):
                    nk = (lt + 1) * 128
                    nch = (nk + 511) // 512
                    for c in range(nch):
                        w = min(512, nk - c * 512)
                        kb.op("pe", lambda e: e.matmul(sbanks[c][0][:, 0:w], lhsT=CT[:, g, lt * 128:(lt + 1) * 128],
                                                       rhs=BT[:, g, c * 512:c * 512 + w], start=True, stop=True),
                              reads=[ctreg, btreg], writes=[sregs[c]])
                    yt, yr = kb.banks[4]
                    for hl in range(4):
                        h = g * 4 + hl
                        i = cnt % 2
                        cnt += 1
                        al = acst[:, lt, h:h + 1]
                        d0 = lt * 128
                        kb.op("dve", lambda e: e.scalar_tensor_tensor(out=dg[i][:], in0=rows[:, hl, d0:d0 + 128], scalar=-1.0,
                                                                      in1=consts.trineg[:], op0=ALU.mult, op1=ALU.add),
                              reads=[rowreg[hl], consts.reg], writes=[dgreg[i]])
                        kb.op("act", lambda e: e.activation(out=E[i][:, d0:d0 + 128], in_=dg[i][:], func=AF.Exp, bias=al),
                              reads=[dgreg[i], acreg], writes=[Ereg[i]])
                        if lt > 0:
                            kb.op("act", lambda e: e.activation(out=E[i][:, 0:d0], in_=rows[:, hl, 0:d0], func=AF.Exp, scale=-1.0,
                                                                bias=al), reads=[rowreg[hl], acreg], writes=[Ereg[i]])
                        kb.op("dve", lambda e: e.tensor_tensor(out=pbuf[i][:, 0:nk], in0=Sbig[:, 0:nk], in1=E[i][:, 0:nk],
                                                               op=ALU.mult), reads=sregs[:nch] + [Ereg[i]], writes=[pbreg[i]])
                        nkt = lt + 1
                        for k4 in range((nkt + 3) // 4):
                            ks = list(range(k4 * 4, min(nkt, k4 * 4 + 4)))
                            transposes_bf16(kb, consts, [pbuf[i][:, k * 128:(k + 1) * 128] for k in ks], [pbreg[i]],
                                            pT[i][:, ks[0]:ks[-1] + 1, :], [pTreg[i]], "act" if k4 % 2 else "dve")
                        for k in range(nkt):
                            kb.op("pe", lambda e: e.matmul(yt[:, hl * 64:(hl + 1) * 64], lhsT=pT[i][:, k, :],
                                                           rhs=xdt[:, k, h * 64:(h + 1) * 64], start=(k == 0), stop=(k == nkt - 1)),
                                  reads=[pTreg[i], xdtreg], writes=[yr])
                    j = cg % 2
                    cg += 1
                    kb.dma("sp", xz[j][:, 0, :], xs_d[lt][:, g * 256:(g + 1) * 256], writes=[xzreg[j]])
                    kb.dma("sp", xz[j][:, 1, :], zs_d[lt][:, g * 256:(g + 1) * 256], writes=[xzreg[j]])
                    kb.op("dve", lambda e: e.tensor_tensor(out=y1[j][:], in0=xz[j][:, 0, :], in1=sp.dsk[:, g * 256:(g + 1) * 256],
                                                           op=ALU.mult), reads=[xzreg[j], consts.reg], writes=[y1reg[j]])
                    kb.op("dve", lambda e: e.tensor_tensor(out=y1[j][:], in0=y1[j][:], in1=yt[:, 0:256], op=ALU.add),
                          reads=[y1reg[j], yr], writes=[y1reg[j]])
                    kb.op("dve", lambda e: e.tensor_tensor(out=y1[j][:], in0=y1[j][:], in1=xz[j][:, 1, :], op=ALU.mult),
                          reads=[y1reg[j], xzreg[j]], writes=[y1reg[j]])
                    kb.op("dve", lambda e: e.tensor_tensor(out=yb[j][:], in0=y1[j][:], in1=y1[j][:], op=ALU.mult),
                          reads=[y1reg[j]], writes=[ybreg[j]])
                    kb.op("dve", lambda e: e.reduce_sum(out=ss[:, j:j + 1], in_=yb[j][:], axis=AX.X), reads=[ybreg[j]], writes=[ssreg])
                    kb.op("dve", lambda e: e.tensor_scalar(out=ss[:, j:j + 1], in0=ss[:, j:j + 1], scalar1=1.0 / 256, scalar2=RMS_EPS,
                                                           op0=ALU.mult, op1=ALU.add), reads=[ssreg], writes=[ssreg])
                    kb.op("act", lambda e: e.activation(out=ss[:, j:j + 1], in_=ss[:, j:j + 1], func=AF.Sqrt), reads=[ssreg], writes=[ssreg])
                    kb.op("dve", lambda e: e.reciprocal(out=ss[:, 2 + j:3 + j], in_=ss[:, j:j + 1]), reads=[ssreg], writes=[ssreg])
                    kb.op("dve", lambda e: e.scalar_tensor_tensor(out=yb[j][:], in0=y1[j][:], scalar=ss[:, 2 + j:3 + j],
                                                                  in1=sp.nw[:, g * 256:(g + 1) * 256], op0=ALU.mult, op1=ALU.mult),
                          reads=[y1reg[j], ssreg, consts.reg], writes=[ybreg[j]])
                    for q in range(2):
                        transposes_bf16(kb, consts, [yb[j][:, q * 128:(q + 1) * 128]], [ybreg[j]],
                                        YS[:, 2 * g + q:2 * g + q + 1, lt * 128:(lt + 1) * 128], [ysreg], "act")
            kb.bank_pool = list(range(8))
            for c in range(8):
                kb.dma("sp", YSd[c], YS[:, c, :], reads=[ysreg])
            kb.barrier()

    return phase_a, phase_b


def even_mixer(kb, XF, R, Win, Wout, consts, sp=None, li=0, scr=None):
    nc = kb.nc
    with ExitStack() as es:
        phase_a, phase_b = ssd_branch(kb, es, Win, sp, li, consts, scr)
        with ExitStack() as esx:
            XB = sbt(nc, esx, "XB", [P, DC, S], BF16)
            xreg = Reg()
            load_xb(kb, XB, xreg, XF)
            phase_a(XB, xreg)
            kb.barrier()
        phase_b(scr["yd"][8:16])
    kb.barrier()
    with ExitStack() as esx:
        XB = sbt(nc, esx, "XB", [P, DC, S], BF16)
        xreg = Reg()
        load_xb(kb, XB, xreg, XF)
        YA = sbt(nc, esx, "YA", [P, 8, S], BF16)
        yareg = Reg()
        attn_heads(kb, XB, xreg, YA, yareg, Win, lambda h: (h * 128, 1024 + h * 128, 2048 + h * 128), 8, "dil", consts, 0)
        for c in range(8):
            kb.dma("sp", scr["yd"][c], YA[:, c, :], reads=[yareg])
        kb.barrier()
    with ExitStack() as es:
        YB = sbt(nc, es, "YB", [P, DC, S], BF16)
        yreg = Reg()
        for c in range(DC):
            kb.dma("sp", YB[:, c, :], scr["yd"][c], writes=[yreg])
        out_proj(kb, XF, R, YB, yreg, Wout)
    kb.barrier()


def load_consts(kb, es, aps):
    nc = kb.nc
    c = Ctx()
    c.ones_f = sbt(nc, es, "ones_f", [P, P], F32)
    c.reg = Reg()
    kb.op("dve", lambda e: e.memset(c.ones_f[:], 1.0), writes=[c.reg])
    c.gb = sbt(nc, es, "gb", [P, 8, 2, DC], F32)
    kb.dma("sp", c.gb[:], aps["ln_gb"], writes=[c.reg])
    c.ident_b = sbt(nc, es, "ident_b", [P, P], BF16)
    kb.dma("pool", c.ident_b[:], aps["cst_ident"], writes=[c.reg])
    c.causal = sbt(nc, es, "causal", [P, 2, 256], BF16)
    kb.dma("pool", c.causal[:], aps["cst_causal"], writes=[c.reg])
    c.gdil = sbt(nc, es, "gdil", [P, 2048], BF16)
    kb.dma("pool", c.gdil[:], aps["cst_gdil"], writes=[c.reg])
    c.ident_f = sbt(nc, es, "ident_f", [P, P], F32)
    kb.dma("sp", c.ident_f[:], aps["cst_ident"], writes=[c.reg])
    c.trineg = sbt(nc, es, "trineg", [P, P], F32)
    kb.dma("sp", c.trineg[:], aps["cst_trineg"], writes=[c.reg])
    c.negmask = sbt(nc, es, "negmask", [P, 128], F32)
    kb.dma("sp", c.negmask[:], aps["cst_negmask"], writes=[c.reg])
    kb.barrier()
    return c


def build(stages, dbg=False):
    nc = bass.Bass("TRN2", target_bir_lowering=False)
    aps = {}

    def din(name, shape, dt=F32):
        aps[name] = nc.dram_tensor(name, list(shape), dt, kind="ExternalInput").ap()
        return aps[name]

    xT = din("xT", [DC, P, S])
    din("ln_gb", [P, 8, 2, DC])
    din("cst_ident", [P, P])
    din("cst_causal", [P, 2, 256])
    din("cst_gdil", [P, 2048])
    din("cst_negmask", [P, 128])
    din("cst_trineg", [P, P])
    kinds = set(st[0] for st in stages)
    if "oddmix" in kinds:
        din("od_w_qkv", [2, D, 3 * D])
        din("od_w_out", [2, D, D])
    if "evenmix" in kinds:
        din("ev_w_in", [2, D, EVEN_IN])
        din("ev_w_out", [2, D, D])
        din("ssd_cw", [P, 2, 16, 4])
        din("ssd_cb", [P, 2, 16])
        din("ssd_dtb", [16, 2])
        din("ssd_alog", [16, 2])
        din("ssd_dsk", [2, 1024])
        din("ssd_nw", [2, 1024])
    if "ffn" in kinds:
        din("ev_ffn_w_gu", [2, D, 2 * FF])
        din("ev_ffn_w_down", [2, FF, D])
    if "moe" in kinds:
        din("od_exp_w_gu", [2, NE, D, 2 * FF])
        din("od_exp_w_down", [2, NE, FF, D])
        din("rt_w", [P, 2, DC, NE])
        din("rt_b", [P, 2, NE])
    out = nc.dram_tensor("outT", [DC, P, S], F32, kind="ExternalOutput").ap()
    XF1 = nc.dram_tensor("XF1", [DC, P, S], F32, kind="Internal").ap()
    R = nc.dram_tensor("Rscr", [DC, P, S], F32, kind="Internal").ap()
    Gd = nc.dram_tensor("Gd", [NE, S], F32, kind="Internal").ap()
    with ExitStack() as es:
        kb = KB(nc, es)
        consts = load_consts(kb, es, aps)
        if "moe" in kinds:
            consts.rt_w = sbt(nc, es, "rt_w", [P, 2, DC, NE], F32)
            consts.rt_b = sbt(nc, es, "rt_b", [P, 2, NE], F32)
            kb.dma("sp", consts.rt_w[:], aps["rt_w"], writes=[consts.reg])
            kb.dma("sp", consts.rt_b[:], aps["rt_b"], writes=[consts.reg])
            kb.barrier()
        scr = None
        if "evenmix" in kinds:
            skind = "ExternalOutput" if os.environ.get("DBG_SSD") else "Internal"
            scr = {"xs_d": nc.dram_tensor("xs_d", [16, P, 1024], BF16, kind=skind).ap(),
                   "zs_d": nc.dram_tensor("zs_d", [16, P, 1024], BF16, kind=skind).ap(),
                   "acs_d": nc.dram_tensor("acs_d", [16, S], F32, kind=skind).ap(),
                   "yd": nc.dram_tensor("yd", [DC, P, S], BF16, kind=skind).ap()}
            sp = Ctx()
            sp.cw = sbt(nc, es, "ssd_cw", [P, 2, 16, 4], F32)
            sp.cb = sbt(nc, es, "ssd_cb", [P, 2, 16], F32)
            sp.dtb = sbt(nc, es, "ssd_dtb", [16, 2], F32)
            sp.alog = sbt(nc, es, "ssd_alog", [16, 2], F32)
            sp.dsk = sbt(nc, es, "ssd_dsk", [P, 1024], F32)
            sp.nw = sbt(nc, es, "ssd_nw", [P, 1024], F32)
            kb.dma("sp", sp.cw[:], aps["ssd_cw"], writes=[consts.reg])
            kb.dma("sp", sp.cb[:], aps["ssd_cb"], writes=[consts.reg])
            kb.dma("sp", sp.dtb[:], aps["ssd_dtb"], writes=[consts.reg])
            kb.dma("sp", sp.alog[:], aps["ssd_alog"], writes=[consts.reg])
            kb.barrier()
        cur = xT
        for si, st in enumerate(stages):
            kind, i, slot = st[0], st[1], st[2]
            last = si == len(stages) - 1
            if kind == "oddmix":
                odd_mixer(kb, cur, R, aps["od_w_qkv"][i], aps["od_w_out"][i], consts)
            elif kind == "evenmix":
                kb.dma("sp", sp.dsk[:], aps["ssd_dsk"][i].partition_broadcast(P), writes=[consts.reg])
                kb.dma("sp", sp.nw[:], aps["ssd_nw"][i].partition_broadcast(P), writes=[consts.reg])
                kb.barrier()
                even_mixer(kb, cur, R, aps["ev_w_in"][i], aps["ev_w_out"][i], consts, sp=sp, li=i, scr=scr)
            elif kind == "ffn":
                ffn_dense(kb, cur, R, aps["ev_ffn_w_gu"][i], aps["ev_ffn_w_down"][i], consts)
            elif kind == "moe":
                moe_dense(kb, cur, R, Gd, aps["od_exp_w_gu"][i], aps["od_exp_w_down"][i], consts.rt_w[:, i], consts.rt_b[:, i],
                          consts)
            ln_phase(kb, R, out if last else XF1, consts.gb, slot, consts)
            cur = XF1
        kb.barrier()
    return nc


def prep_ln(inputs):
    arr = np.zeros((P, 8, 2, DC), np.float32)
    for layer in range(DEPTH):
        i = layer // 2
        pre = "ev" if layer % 2 == 0 else "od"
        for j, nm in enumerate(["ln1", "ln2"]):
            g = np.asarray(inputs["%s_%s_g" % (pre, nm)][i], np.float32).reshape(DC, P).T
            b = np.asarray(inputs["%s_%s_b" % (pre, nm)][i], np.float32).reshape(DC, P).T
            arr[:, layer * 2 + j, 0, :] = g
            arr[:, layer * 2 + j, 1, :] = b
    return arr


def make_consts():
    c = {}
    c["cst_ident"] = np.eye(P, dtype=np.float32)
    ql = np.arange(P)[:, None]
    kl = np.arange(256)[None, :]
    causal = np.zeros((P, 2, 256), np.float32)
    causal[:, 0, :] = (kl <= ql)
    causal[:, 1, :] = (kl <= ql + 128)
    c["cst_causal"] = causal
    u = np.arange(2048)[None, :]
    delta = 1920 + ql - u
    mult = ((delta >= 0) & (delta <= 128)).astype(np.float32)
    mult += ((delta >= 0) & (delta % 4 == 0) & (delta // 4 <= 128)).astype(np.float32)
    mult += ((delta >= 0) & (delta % 16 == 0) & (delta // 16 <= 128)).astype(np.float32)
    c["cst_gdil"] = mult.astype(np.float32)
    nm = np.zeros((P, 16, 8), np.float32)
    for qt in range(16):
        for j in range(8):
            if j >= qt // 2:
                nm[:, qt, j] = -1e30
    c["cst_negmask"] = nm.reshape(P, 128)
    sl = np.arange(P)[None, :]
    c["cst_trineg"] = np.where(sl <= ql, 0.0, -30000.0).astype(np.float32)
    return c


def prep_ssd(inputs):
    o = {}
    cw = np.asarray(inputs["ev_conv_w"], np.float32)
    o["ssd_cw"] = np.ascontiguousarray(cw.reshape(2, 4, 16, P).transpose(3, 0, 2, 1))
    cb = np.asarray(inputs["ev_conv_b"], np.float32)
    o["ssd_cb"] = np.ascontiguousarray(cb.reshape(2, 16, P).transpose(2, 0, 1))
    o["ssd_dtb"] = np.ascontiguousarray(np.asarray(inputs["ev_dt_bias"], np.float32).T)
    o["ssd_alog"] = np.ascontiguousarray(np.asarray(inputs["ev_a_log"], np.float32).T)
    o["ssd_dsk"] = np.ascontiguousarray(np.repeat(np.asarray(inputs["ev_d_skip"], np.float32), 64, axis=1))
    o["ssd_nw"] = np.ascontiguousarray(np.asarray(inputs["ev_ssd_norm"], np.float32))
    return o


FULL_STAGES = [("evenmix", 0, 0), ("ffn", 0, 1), ("oddmix", 0, 2), ("moe", 0, 3),
               ("evenmix", 1, 4), ("ffn", 1, 5), ("oddmix", 1, 6), ("moe", 1, 7)]


def kernel(**inputs):
    x = np.asarray(inputs["x"], np.float32)
    nc = build(FULL_STAGES)
    shared = make_consts()
    shared["ln_gb"] = prep_ln(inputs)
    shared.update(prep_ssd(inputs))
    for k in ["od_w_qkv", "od_w_out", "ev_w_in", "ev_w_out", "ev_ffn_w_gu", "ev_ffn_w_down", "od_exp_w_gu", "od_exp_w_down"]:
        shared[k] = np.ascontiguousarray(np.asarray(inputs[k], np.float32))
    rw = np.asarray(inputs["od_router_w"], np.float32)
    shared["rt_w"] = np.ascontiguousarray(rw.reshape(2, DC, P, NE).transpose(2, 0, 1, 3))
    rb = np.asarray(inputs["od_router_b"], np.float32)
    shared["rt_b"] = np.ascontiguousarray(np.broadcast_to(rb[None], (P, 2, NE)))
    in_maps = []
    for c in range(NCORES):
        m = dict(shared)
        m["xT"] = np.ascontiguousarray(x[c].T).reshape(DC, P, S)
        in_maps.append(m)
    res = run_bass_kernel_spmd(nc, in_maps, core_ids=list(range(NCORES)))
    out = np.stack([r["outT"].reshape(D, S).T for r in res.results], axis=0)
    return np.ascontiguousarray(out.astype(np.float32))
```

```python
import os
import numpy as np
import concourse.bass as bass
import concourse.mybir as mybir
from concourse.bass_utils import run_bass_kernel_spmd
from contextlib import ExitStack

F32 = mybir.dt.float32
BF16 = mybir.dt.bfloat16
I32 = mybir.dt.int32
AF = mybir.ActivationFunctionType
ALU = mybir.AluOpType
AX = mybir.AxisListType

D = 2048
S = 2048
P = 128
DC = 16
FF = 5632
FC = 44
NE = 8
DEPTH = 4
ALPHA = float((2 * DEPTH) ** 0.25)
LN_EPS = 1e-5
RMS_EPS = 1e-5
EVEN_IN = 6160
NCORES = 8
CAP = 640
NST = CAP // 128


class Reg:
    __slots__ = ("w", "r", "name", "excl")

    def __init__(self, name="", excl=False):
        self.w = None
        self.r = []
        self.name = name
        self.excl = excl


class DSlot:
    __slots__ = ("sem", "count", "last")

    def __init__(self, sem):
        self.sem = sem
        self.count = 0
        self.last = None


class KB:
    def __init__(self, nc, es, n_dsem=20):
        self.nc = nc
        self.es = es
        self.engs = dict(pe=nc.tensor, act=nc.scalar, dve=nc.vector, pool=nc.gpsimd, sp=nc.sync)
        self.sem = {e: es.enter_context(nc.semaphore("s_" + e)) for e in ["pe", "act", "dve", "pool"]}
        self.cnt = {e: 0 for e in self.sem}
        self.waited = {e: {} for e in self.engs}
        self.dslots = {q: [DSlot(es.enter_context(nc.semaphore("d_%s%d" % (q, i)))) for i in range(n_dsem)]
                       for q in ["sp", "pool"]}
        self.dnext = {"sp": 0, "pool": 0}
        self.nbank = 0
        self.bank_pool = list(range(8))
        self.banks = []
        self.pbig = [es.enter_context(nc.psum_tensor("pbig%d" % i, [P, 2048], F32)) for i in range(2)]
        for i in range(8):
            t = self.pbig[i // 4][:, (i % 4) * 512:(i % 4 + 1) * 512]
            self.banks.append((t, Reg("bank%d" % i, excl=True)))

    def bank(self):
        pool = self.bank_pool
        b = self.banks[pool[self.nbank % len(pool)]]
        self.nbank += 1
        return b

    def _wait(self, eng, tok):
        sem, val = tok
        if eng == "pe" and sem is self.sem["pe"]:
            return
        k = id(sem)
        if self.waited[eng].get(k, 0) < val:
            self.engs[eng].wait_ge(sem, val)
            self.waited[eng][k] = val

    def _deps(self, eng, reads, writes):
        deps = []
        for r in reads:
            if r.w is not None:
                deps.append(r.w)
        for w in writes:
            if w.w is not None:
                deps.append(w.w)
            deps.extend(w.r)
        for t in deps:
            self._wait(eng, t)

    def _commit(self, tok, reads, writes):
        for r in reads:
            r.r.append(tok)
            if len(r.r) > 64:
                best = {}
                for (s, v) in r.r:
                    if id(s) not in best or best[id(s)][1] < v:
                        best[id(s)] = (s, v)
                r.r = list(best.values())
        for w in writes:
            w.w = tok
            w.r = []

    def op(self, eng, fn, reads=(), writes=()):
        ex = [r for r in reads if r.excl]
        if ex:
            reads = [r for r in reads if not r.excl]
            writes = list(writes) + ex
        self._deps(eng, reads, writes)
        ins = fn(self.engs[eng])
        self.cnt[eng] += 1
        ins.then_inc(self.sem[eng], 1)
        tok = (self.sem[eng], self.cnt[eng])
        self._commit(tok, reads, writes)
        return tok

    def dma(self, q, out, in_, reads=(), writes=(), **kw):
        slots = self.dslots[q]
        sl = slots[self.dnext[q] % len(slots)]
        self.dnext[q] += 1
        if sl.last is not None:
            self._wait(q, sl.last)
        self._deps(q, reads, writes)
        ins = self.engs[q].dma_start(out=out, in_=in_, **kw)
        sl.count += 16
        ins.then_inc(sl.sem, 16)
        tok = (sl.sem, sl.count)
        sl.last = tok
        self._commit(tok, reads, writes)
        return tok

    def barrier(self):
        toks = [(self.sem[e], self.cnt[e]) for e in self.sem if self.cnt[e] > 0]
        for q in self.dslots:
            for sl in self.dslots[q]:
                if sl.last is not None:
                    toks.append(sl.last)
        for e in self.engs:
            for t in toks:
                self._wait(e, t)


def r3(W, p=P):
    return W.rearrange("(k p) c -> p k c", p=p)


class Ctx:
    pass


_UNIQ = [0]


def sbt(nc, es, name, shape, dt):
    _UNIQ[0] += 1
    return es.enter_context(nc.sbuf_tensor("%s_%d" % (name, _UNIQ[0]), list(shape), dt))


def stream_gemm_fm(kb, es_outer, W, KC, col_chunks, rhs_fn, rhs_regs, NTB, evac, slab=1, name="w", extra_cols=None):
    nc = kb.nc
    with ExitStack() as es:
        wb = [sbt(nc, es, "%s_wb%d" % (name, i), [P, KC, slab * 128], BF16) for i in range(2)]
        wr = [Reg(), Reg()]
        W3 = r3(W)
        ngroups = (len(col_chunks) + slab - 1) // slab
        for g in range(ngroups):
            chunks = col_chunks[g * slab:(g + 1) * slab]
            c0 = chunks[0]
            n = len(chunks)
            b = g % 2
            kb.dma("pool", wb[b][:, :, 0:n * 128], W3[:, :, c0:c0 + n * 128], writes=[wr[b]])
            for j, cc in enumerate(chunks):
                ci = g * slab + j
                for tb in range(NTB):
                    bt, br = kb.bank()
                    for kc in range(KC):
                        kb.op("pe", lambda e, kc=kc, j=j, tb=tb: e.matmul(
                            bt, lhsT=wb[b][:, kc, j * 128:(j + 1) * 128], rhs=rhs_fn(kc, tb),
                            start=(kc == 0), stop=(kc == KC - 1)),
                            reads=[wr[b]] + list(rhs_regs), writes=[br])
                    evac(ci, tb, bt, br)
        kb.barrier()


def stream_gemm_hw(kb, W, KC, col_groups, rhs_fn, rhs_regs, NTB, evac, name="w", ksplit=1):
    nc = kb.nc
    ncs = col_groups[0][1]
    KQ = KC // ksplit
    assert KQ * ksplit == KC
    W3 = r3(W)
    with ExitStack() as es:
        stg = [sbt(nc, es, "%s_st%d" % (name, i), [P, KQ, ncs * 128], F32) for i in range(2)]
        sreg = [Reg(), Reg()]
        wb = [sbt(nc, es, "%s_wb%d" % (name, i), [P, KQ, ncs * 128], BF16) for i in range(2)]
        wr = [[Reg(), Reg()], [Reg(), Reg()]]
        items = [(g, q) for g in range(len(col_groups)) for q in range(ksplit)]
        h1 = KQ // 2

        def dma(idx):
            g, q = items[idx]
            c0, n = col_groups[g]
            kb.dma("sp", stg[idx % 2][:, :, 0:n * 128], W3[:, q * KQ:(q + 1) * KQ, c0:c0 + n * 128], writes=[sreg[idx % 2]])

        def cast(idx):
            u = idx % 2
            kb.op("act", lambda e: e.activation(out=wb[u][:, 0:h1, :], in_=stg[u][:, 0:h1, :], func=AF.Copy),
                  reads=[sreg[u]], writes=[wr[u][0]])
            kb.op("dve", lambda e: e.tensor_copy(out=wb[u][:, h1:KQ, :], in_=stg[u][:, h1:KQ, :]),
                  reads=[sreg[u]], writes=[wr[u][1]])

        dma(0)
        if len(items) > 1:
            dma(1)
        cast(0)
        banks = {}
        for idx in range(len(items)):
            g, q = items[idx]
            u = idx % 2
            if idx + 1 < len(items):
                cast(idx + 1)
            for j in range(ncs):
                for tb in range(NTB):
                    if q == 0:
                        banks[(j, tb)] = kb.bank()
                    bt, br = banks[(j, tb)]
                    for kk in range(KQ):
                        kc = q * KQ + kk
                        kb.op("pe", lambda e: e.matmul(bt, lhsT=wb[u][:, kk, j * 128:(j + 1) * 128], rhs=rhs_fn(kc, tb),
                                                       start=(kc == 0), stop=(kc == KC - 1)),
                              reads=[wr[u][0 if kk < h1 else 1]] + list(rhs_regs), writes=[br])
                    if q == ksplit - 1:
                        evac(g * ncs + j, tb, bt, br)
            if idx + 2 < len(items):
                dma(idx + 2)
        kb.barrier()


def ln_phase(kb, R, XF, gb, li, consts):
    nc = kb.nc
    with ExitStack() as es:
        rbuf = [sbt(nc, es, "ln_r%d" % i, [P, S], F32) for i in range(2)]
        rreg = [Reg(), Reg()]
        sq = [sbt(nc, es, "ln_sq%d" % i, [P, S], F32) for i in range(2)]
        sqreg = [Reg(), Reg()]
        mean = sbt(nc, es, "ln_mean", [P, S], F32)
        rstd = sbt(nc, es, "ln_rstd", [P, S], F32)
        msq = sbt(nc, es, "ln_msq", [P, S], F32)
        streg = Reg()
        banks = [kb.bank() for _ in range(8)]
        for dc in range(DC):
            b = dc % 2
            kb.dma("sp", rbuf[b][:], R[dc], writes=[rreg[b]])
            kb.op("act", lambda e: e.activation(out=sq[b][:], in_=rbuf[b][:], func=AF.Square),
                  reads=[rreg[b]], writes=[sqreg[b]])
            for tb in range(4):
                kb.op("pe", lambda e: e.matmul(banks[tb][0], lhsT=consts.ones_f[:], rhs=rbuf[b][:, tb * 512:(tb + 1) * 512],
                                               start=(dc == 0), stop=(dc == DC - 1)),
                      reads=[rreg[b]], writes=[banks[tb][1]])
                kb.op("pe", lambda e: e.matmul(banks[4 + tb][0], lhsT=consts.ones_f[:], rhs=sq[b][:, tb * 512:(tb + 1) * 512],
                                               start=(dc == 0), stop=(dc == DC - 1)),
                      reads=[sqreg[b]], writes=[banks[4 + tb][1]])
        for tb in range(4):
            sl = slice(tb * 512, (tb + 1) * 512)
            kb.op("dve", lambda e: e.tensor_scalar(out=mean[:, sl], in0=banks[tb][0], scalar1=1.0 / D, scalar2=None,
                                                   op0=ALU.mult), reads=[banks[tb][1]], writes=[streg])
            kb.op("dve", lambda e: e.tensor_tensor(out=msq[:, sl], in0=mean[:, sl], in1=mean[:, sl], op=ALU.mult),
                  reads=[streg], writes=[streg])
            kb.op("dve", lambda e: e.scalar_tensor_tensor(out=rstd[:, sl], in0=banks[4 + tb][0], scalar=1.0 / D,
                                                          in1=msq[:, sl], op0=ALU.mult, op1=ALU.subtract),
                  reads=[banks[4 + tb][1], streg], writes=[streg])
            kb.op("dve", lambda e: e.tensor_scalar(out=rstd[:, sl], in0=rstd[:, sl], scalar1=LN_EPS, scalar2=None,
                                                   op0=ALU.add), reads=[streg], writes=[streg])
            kb.op("act", lambda e: e.activation(out=rstd[:, sl], in_=rstd[:, sl], func=AF.Sqrt),
                  reads=[streg], writes=[streg])
            kb.op("dve", lambda e: e.reciprocal(out=rstd[:, sl], in_=rstd[:, sl]), reads=[streg], writes=[streg])
        for dc in range(DC):
            b = dc % 2
            kb.dma("sp", rbuf[b][:], R[dc], writes=[rreg[b]])
            kb.op("dve", lambda e: e.tensor_tensor(out=sq[b][:], in0=rbuf[b][:], in1=mean[:], op=ALU.subtract),
                  reads=[rreg[b], streg], writes=[sqreg[b]])
            kb.op("pool", lambda e: e.tensor_tensor(out=sq[b][:], in0=sq[b][:], in1=rstd[:], op=ALU.mult),
                  reads=[sqreg[b], streg], writes=[sqreg[b]])
            kb.op("act", lambda e: e.activation(out=sq[b][:], in_=sq[b][:], func=AF.Identity,
                                                scale=gb[:, li, 0, dc:dc + 1], bias=gb[:, li, 1, dc:dc + 1]),
                  reads=[sqreg[b]], writes=[sqreg[b]])
            kb.dma("sp", XF[dc], sq[b][:], reads=[sqreg[b]])
        kb.barrier()


def ffn_dense(kb, XF, R, Wgu, Wdown, consts, gate_dram=None, acc=False):
    nc = kb.nc
    TB = 1024
    for pss in range(S // TB):
        t0 = pss * TB
        with ExitStack() as es:
            xb = sbt(nc, es, "ffn_xb", [P, DC, TB], BF16)
            xreg = [Reg() for _ in range(DC)]
            at = sbt(nc, es, "ffn_at", [P, FC, TB], BF16)
            areg = [Reg() for _ in range(FC)]
            for dc in range(DC):
                kb.dma("pool", xb[:, dc, :], XF[dc][:, t0:t0 + TB], writes=[xreg[dc]])
            gate = None
            if gate_dram is not None:
                gt = sbt(nc, es, "ffn_gate", [P, TB], F32)
                greg = Reg()
                kb.dma("sp", gt[:], gate_dram[t0:t0 + TB].partition_broadcast(P), writes=[greg])
                gate = (gt, greg)
            NTB = TB // 512
            sg = [sbt(nc, es, "ffn_sg%d" % i, [P, 512], F32) for i in range(2 * NTB)]
            sgreg = [Reg() for _ in range(2 * NTB)]

            def evac_up(ci, tb, bt, br):
                gi, j = ci // 2, ci % 2
                fc = 2 * (gi // 2) + j
                i = j * NTB + tb
                if gi % 2 == 0:
                    kb.op("act", lambda e: e.activation(out=sg[i][:], in_=bt, func=AF.Silu), reads=[br], writes=[sgreg[i]])
                else:
                    kb.op("dve", lambda e: e.tensor_tensor(out=at[:, fc, tb * 512:(tb + 1) * 512], in0=sg[i][:], in1=bt,
                                                           op=ALU.mult), reads=[br, sgreg[i]], writes=[areg[fc]])

            groups = []
            for fp in range(FC // 2):
                groups += [(fp * 256, 2), (FF + fp * 256, 2)]
            stream_gemm_hw(kb, Wgu, DC, groups, lambda kc, tb: xb[:, kc, tb * 512:(tb + 1) * 512], xreg, NTB, evac_up, name="gu")
            evac_down = resid_evac_factory(kb, es, R if acc else XF, R, t0, TB, "ffn", alpha=(1.0 if acc else ALPHA), gate=gate)
            stream_gemm_hw(kb, Wdown, FC, [(dp * 256, 2) for dp in range(DC // 2)],
                           lambda kc, tb: at[:, kc, tb * 512:(tb + 1) * 512], areg, NTB, evac_down, name="dn", ksplit=4)
        kb.barrier()


def moe_router(kb, XF, Gd, wr_sb, br_sb, consts):
    nc = kb.nc
    with ExitStack() as es:
        xt = [sbt(nc, es, "rt_x%d" % i, [P, DC, P], F32) for i in range(2)]
        xtreg = [Reg(), Reg()]
        lg = sbt(nc, es, "rt_lg", [P, 16, 8], F32)
        t8 = sbt(nc, es, "rt_t8", [P, 16, 8], F32)
        g1 = sbt(nc, es, "rt_g1", [P, 16, 2], F32)
        m1 = sbt(nc, es, "rt_m1", [P, 16, 8], F32)
        m2 = sbt(nc, es, "rt_m2", [P, 16, 8], F32)
        lreg = Reg()
        for tt in range(16):
            b = tt % 2
            with nc.allow_non_contiguous_dma(reason="router token tile"):
                kb.dma("sp", xt[b][:], XF[:, :, tt * 128:(tt + 1) * 128].rearrange("c p t -> p c t"), writes=[xtreg[b]])
            bt, br = kb.bank()
            for dc in range(DC):
                kb.op("pe", lambda e: e.matmul(bt[:, 0:8], lhsT=xt[b][:, dc, :], rhs=wr_sb[:, dc, :], start=(dc == 0),
                                               stop=(dc == DC - 1)), reads=[xtreg[b], consts.reg], writes=[br])
            kb.op("dve", lambda e: e.tensor_tensor(out=lg[:, tt, :], in0=bt[:, 0:8], in1=br_sb[:], op=ALU.add),
                  reads=[br, consts.reg], writes=[lreg])
            kb.op("dve", lambda e: e.max(out=t8[:, tt, :], in_=lg[:, tt, :]), reads=[lreg], writes=[lreg])
            kb.op("dve", lambda e: e.tensor_tensor(out=g1[:, tt, 0:1], in0=t8[:, tt, 0:1], in1=t8[:, tt, 1:2], op=ALU.subtract),
                  reads=[lreg], writes=[lreg])
            kb.op("act", lambda e: e.activation(out=g1[:, tt, 0:1], in_=g1[:, tt, 0:1], func=AF.Sigmoid), reads=[lreg], writes=[lreg])
            kb.op("dve", lambda e: e.tensor_scalar(out=g1[:, tt, 1:2], in0=g1[:, tt, 0:1], scalar1=-1.0, scalar2=1.0,
                                                   op0=ALU.mult, op1=ALU.add), reads=[lreg], writes=[lreg])
            kb.op("dve", lambda e: e.tensor_scalar(out=m1[:, tt, :], in0=lg[:, tt, :], scalar1=t8[:, tt, 0:1], scalar2=g1[:, tt, 0:1],
                                                   op0=ALU.is_equal, op1=ALU.mult), reads=[lreg], writes=[lreg])
            kb.op("dve", lambda e: e.tensor_scalar(out=m2[:, tt, :], in0=lg[:, tt, :], scalar1=t8[:, tt, 1:2], scalar2=g1[:, tt, 1:2],
                                                   op0=ALU.is_equal, op1=ALU.mult), reads=[lreg], writes=[lreg])
            kb.op("dve", lambda e: e.tensor_tensor(out=m1[:, tt, :], in0=m1[:, tt, :], in1=m2[:, tt, :], op=ALU.add),
                  reads=[lreg], writes=[lreg])
            with nc.allow_non_contiguous_dma(reason="gate transpose store"):
                kb.dma("sp", Gd[:, tt * 128:(tt + 1) * 128].rearrange("e t -> t e"), m1[:, tt, :], reads=[lreg])
        kb.barrier()


def moe_dense(kb, XF, R, Gd, Wgu8, Wdown8, wr_sb, br_sb, consts):
    moe_router(kb, XF, Gd, wr_sb, br_sb, consts)
    for e_ in range(NE):
        ffn_dense(kb, XF, R, Wgu8[e_], Wdown8[e_], consts, gate_dram=Gd[e_], acc=(e_ > 0))


SCALE = float(128 ** -0.5)


def transposes_bf16(kb, consts, srcs, src_regs, dst3, dst_regs, eng):
    bt, br = kb.bank()
    bb = bt.bitcast(BF16)
    n = len(srcs)
    for j, sap in enumerate(srcs):
        kb.op("pe", lambda e: e.transpose(out=bb[:, j * 128:(j + 1) * 128], in_=sap, identity=consts.ident_b[:]),
              reads=list(src_regs) + [consts.reg], writes=[br])
    src3 = bb[:, 0:n * 128].rearrange("p (a b) -> p a b", b=128)
    if eng == "act":
        kb.op("act", lambda e: e.activation(out=dst3, in_=src3, func=AF.Copy), reads=[br], writes=list(dst_regs))
    else:
        kb.op("dve", lambda e: e.tensor_copy(out=dst3, in_=src3), reads=[br], writes=list(dst_regs))


def attn_heads(kb, XB, xreg, YB, yreg, W, cols_fn, nheads, mode, consts, ych0):
    nc = kb.nc
    moba = mode == "moba"
    with ExitStack() as es:
        qb = sbt(nc, es, "qb", [P, S], BF16)
        kbf = sbt(nc, es, "kbf", [P, S], BF16)
        vT = sbt(nc, es, "vT", [P, S], BF16)
        qreg, kreg, vreg = Reg(), Reg(), Reg()
        vtok = sbt(nc, es, "vtok", [P, 16, 132], BF16)
        vtreg = Reg()
        pbuf = [sbt(nc, es, "pb%d" % i, [P, S], BF16) for i in range(2)]
        pbreg = [Reg(), Reg()]
        pT = [sbt(nc, es, "pT%d" % i, [P, 16, P], BF16) for i in range(2)]
        pTreg = [Reg(), Reg()]
        on = [sbt(nc, es, "on%d" % i, [P, P], BF16) for i in range(2)]
        onreg = [Reg(), Reg()]
        mx2 = sbt(nc, es, "mx", [P, 2, 4], F32)
        mxregs = [Reg(), Reg()]
        if moba:
            qf = sbt(nc, es, "qf", [P, S], F32)
            qfreg = Reg()
            ksum = sbt(nc, es, "ksum", [P, 8], F32)
            ksreg = Reg()
            gm = sbt(nc, es, "gm", [P, 16, 8], F32)
            top8 = sbt(nc, es, "top8", [P, 16, 8], F32)
            selb = sbt(nc, es, "selb", [P, 16, 8], F32)
            bj = sbt(nc, es, "bj", [P, 2, 8], F32)
            gmreg = Reg()
            bjreg = [Reg(), Reg()]
        kb.op("dve", lambda e: e.memset(vtok[:, :, 128:129], 1.0), writes=[vtreg])
        sbanks = [kb.banks[i] for i in range(4)]
        sregs_all = [b[1] for b in sbanks]
        kb.bank_pool = [4, 5, 6, 7]
        ctr = {'cnt': 0, 'scnt': 0}
        for h in range(int(os.environ.get('ATT_HEADS', nheads))):
            qc, kc_, vc = cols_fn(h)

            def evac(ci, tb, bt, br):
                sl = slice(tb * 512, (tb + 1) * 512)
                if ci == 0:
                    kb.op("act", lambda e: e.activation(out=qb[:, sl], in_=bt, func=AF.Copy), reads=[br], writes=[qreg])
                    if moba:
                        kb.op("dve", lambda e: e.tensor_copy(out=qf[:, sl], in_=bt), reads=[br], writes=[qfreg])
                elif ci == 1:
                    kb.op("act", lambda e: e.activation(out=kbf[:, sl], in_=bt, func=AF.Copy), reads=[br], writes=[kreg])
                    if moba:
                        kb.op("dve", lambda e: e.reduce_sum(out=ksum[:, 2 * tb:2 * tb + 2],
                                                            in_=bt.rearrange("p (b k) -> p b k", k=256), axis=AX.X),
                              reads=[br], writes=[ksreg])
                else:
                    kb.op("dve", lambda e: e.tensor_copy(out=vT[:, sl], in_=bt), reads=[br], writes=[vreg])

            stream_gemm_fm(kb, es, W, DC, [qc, kc_, vc], lambda kc, tb: XB[:, kc, tb * 512:(tb + 1) * 512], xreg, 4,
                           evac, slab=1, name="qkv")
            if os.environ.get("ATT_STOP") == "1":
                continue
            for t4 in range(4):
                transposes_bf16(kb, consts, [vT[:, (t4 * 4 + j) * 128:(t4 * 4 + j + 1) * 128] for j in range(4)], [vreg],
                                vtok[:, t4 * 4:t4 * 4 + 4, 0:128], [vtreg], "act" if t4 % 2 else "dve")
            if os.environ.get("ATT_STOP") == "2":
                continue
            if moba:
                kb.op("dve", lambda e: e.tensor_scalar(out=ksum[:], in0=ksum[:], scalar1=1.0 / 256, scalar2=None, op0=ALU.mult),
                      reads=[ksreg], writes=[ksreg])
                gt, gr = kb.bank()
                for qt in range(16):
                    kb.op("pe", lambda e: e.matmul(gt[:, qt * 8:(qt + 1) * 8], lhsT=qf[:, qt * 128:(qt + 1) * 128], rhs=ksum[:, 0:8],
                                                   start=True, stop=True), reads=[qfreg, ksreg], writes=[gr])
                kb.op("dve", lambda e: e.tensor_tensor(out=gm[:].rearrange("p a b -> p (a b)"), in0=gt[:, 0:128],
                                                       in1=consts.negmask[:], op=ALU.add), reads=[gr, consts.reg], writes=[gmreg])
                for qt in range(16):
                    kb.op("dve", lambda e: e.max(out=top8[:, qt, :], in_=gm[:, qt, :]), reads=[gmreg], writes=[gmreg])
                for qt in range(16):
                    kb.op("dve", lambda e: e.tensor_scalar(out=selb[:, qt, :], in0=gm[:, qt, :], scalar1=top8[:, qt, 2:3],
                                                           scalar2=None, op0=ALU.is_ge), reads=[gmreg], writes=[gmreg])
                kb.op("dve", lambda e: e.tensor_scalar(out=selb[:].rearrange("p a b -> p (a b)"),
                                                       in0=selb[:].rearrange("p a b -> p (a b)"), scalar1=-1.0, scalar2=30000.0,
                                                       op0=ALU.add, op1=ALU.mult), reads=[gmreg], writes=[gmreg])
            def stage_a(qt):
                cnt = ctr['cnt']; scnt = ctr['scnt']
                i = cnt % 2
                cnt += 1
                mx = mx2[:, i, :]
                mxreg = mxregs[i]
                if moba:
                    n = qt // 2
                    nk = (n + 1) * 256
                else:
                    nk = (qt + 1) * 128
                nch = (nk + 511) // 512
                sb0 = 0
                if nch <= 2:
                    sb0 = 2 * (scnt % 2)
                    scnt += 1
                sregs = sregs_all[sb0:sb0 + nch]
                Sbig = kb.pbig[0][:, sb0 * 512:(sb0 + 4) * 512] if sb0 == 0 else kb.pbig[0][:, sb0 * 512:2048]
                for c in range(nch):
                    w = min(512, nk - c * 512)
                    kb.op("pe", lambda e: e.matmul(sbanks[sb0 + c][0][:, 0:w], lhsT=qb[:, qt * 128:(qt + 1) * 128],
                                                   rhs=kbf[:, c * 512:c * 512 + w], start=True, stop=True),
                          reads=[qreg, kreg], writes=[sregs[c]])
                kb.op("dve", lambda e: e.reduce_max(out=mx[:, 0:1], in_=Sbig[:, 0:nk], axis=AX.X),
                      reads=sregs[:nch], writes=[mxreg])
                kb.op("dve", lambda e: e.tensor_scalar(out=mx[:, 1:2], in0=mx[:, 0:1], scalar1=-SCALE, scalar2=None, op0=ALU.mult),
                      reads=[mxreg], writes=[mxreg])
                pb = pbuf[i]
                if moba:
                    if n > 0:
                        kb.op("dve", lambda e: e.tensor_scalar(out=bj[:, i, :], in0=selb[:, qt, :], scalar1=mx[:, 1:2], scalar2=None,
                                                               op0=ALU.add), reads=[gmreg, mxreg], writes=[bjreg[i]])
                    for j in range(n):
                        kb.op("act", lambda e: e.activation(out=pb[:, j * 256:(j + 1) * 256], in_=Sbig[:, j * 256:(j + 1) * 256],
                                                            func=AF.Exp, scale=SCALE, bias=bj[:, i, j:j + 1]),
                              reads=sregs[:nch] + [bjreg[i]], writes=[pbreg[i]])
                    kb.op("act", lambda e: e.activation(out=pb[:, n * 256:(n + 1) * 256], in_=Sbig[:, n * 256:(n + 1) * 256],
                                                        func=AF.Exp, scale=SCALE, bias=mx[:, 1:2]),
                          reads=sregs[:nch] + [mxreg], writes=[pbreg[i]])
                    kb.op("dve", lambda e: e.tensor_tensor(out=pb[:, n * 256:(n + 1) * 256], in0=pb[:, n * 256:(n + 1) * 256],
                                                           in1=consts.causal[:, qt % 2, :], op=ALU.mult),
                          reads=[pbreg[i], consts.reg], writes=[pbreg[i]])
                else:
                    kb.op("act", lambda e: e.activation(out=pb[:, 0:nk], in_=Sbig[:, 0:nk], func=AF.Exp, scale=SCALE,
                                                        bias=mx[:, 1:2]), reads=sregs[:nch] + [mxreg], writes=[pbreg[i]])
                    off = 128 * (15 - qt)
                    kb.op("dve", lambda e: e.tensor_tensor(out=pb[:, 0:nk], in0=pb[:, 0:nk], in1=consts.gdil[:, off:off + nk],
                                                           op=ALU.mult), reads=[pbreg[i], consts.reg], writes=[pbreg[i]])
                ctr['cnt'] = cnt; ctr['scnt'] = scnt
                return dict(i=i, nk=nk, pb=pb)

            def stage_b(qt, st):
                i, nk, pb = st['i'], st['nk'], st['pb']
                mx = mx2[:, i, :]
                mxreg = mxregs[i]
                nkt = nk // 128
                for k4 in range((nkt + 3) // 4):
                    ks = list(range(k4 * 4, min(nkt, k4 * 4 + 4)))
                    transposes_bf16(kb, consts, [pb[:, k * 128:(k + 1) * 128] for k in ks], [pbreg[i]],
                                    pT[i][:, ks[0]:ks[-1] + 1, :], [pTreg[i]], "act" if k4 % 2 else "dve")
                ot, orr = kb.bank()
                for k in range(nkt):
                    kb.op("pe", lambda e: e.matmul(ot[:, 0:129], lhsT=pT[i][:, k, :], rhs=vtok[:, k, 0:129],
                                                   start=(k == 0), stop=(k == nkt - 1)),
                          reads=[pTreg[i], vtreg], writes=[orr])
                kb.op("dve", lambda e: e.reciprocal(out=mx[:, 2:3], in_=ot[:, 128:129]), reads=[orr], writes=[mxreg])
                kb.op("dve", lambda e: e.tensor_scalar(out=on[i][:], in0=ot[:, 0:128], scalar1=mx[:, 2:3], scalar2=None,
                                                       op0=ALU.mult), reads=[orr, mxreg], writes=[onreg[i]])
                transposes_bf16(kb, consts, [on[i][:]], [onreg[i]],
                                YB[:, ych0 + h:ych0 + h + 1, qt * 128:(qt + 1) * 128], [yreg], "act")


            prev = None
            for qt in range(int(os.environ.get('ATT_QT', 16))):
                st = stage_a(qt)
                if prev is not None:
                    stage_b(*prev)
                prev = (qt, st)
            if prev is not None:
                stage_b(*prev)
        kb.bank_pool = list(range(8))
        kb.barrier()


def resid_evac_factory(kb, es, XFin, R, t0, TB, name, alpha=ALPHA, gate=None):
    nc = kb.nc
    xf = [sbt(nc, es, "%s_xf%d" % (name, i), [P, TB], F32) for i in range(2)]
    xfreg = [Reg(), Reg()]
    rb = [sbt(nc, es, "%s_rb%d" % (name, i), [P, 512], F32) for i in range(4)]
    rbreg = [Reg() for _ in range(4)]
    st2 = {"n": 0}

    def evac(ci, tb, bt, br):
        b = ci % 2
        if tb == 0:
            kb.dma("sp", xf[b][:], XFin[ci][:, t0:t0 + TB], writes=[xfreg[b]])
        i = st2["n"] % 4
        st2["n"] += 1
        src = bt
        rds = [br, xfreg[b]]
        if gate is not None:
            kb.op("dve", lambda e: e.tensor_tensor(out=rb[i][:], in0=bt, in1=gate[0][:, tb * 512:(tb + 1) * 512], op=ALU.mult),
                  reads=[br, gate[1]], writes=[rbreg[i]])
            src = rb[i][:]
            rds = [rbreg[i], xfreg[b]]
        kb.op("dve", lambda e: e.scalar_tensor_tensor(out=rb[i][:], in0=xf[b][:, tb * 512:(tb + 1) * 512], scalar=alpha,
                                                      in1=src, op0=ALU.mult, op1=ALU.add),
              reads=rds, writes=[rbreg[i]])
        kb.dma("sp", R[ci][:, t0 + tb * 512:t0 + (tb + 1) * 512], rb[i][:], reads=[rbreg[i]])

    return evac


def out_proj(kb, XFin, R, YB, yreg, Wout):
    with ExitStack() as es:
        evac = resid_evac_factory(kb, es, XFin, R, 0, S, "op")
        stream_gemm_hw(kb, Wout, DC, [(dp * 256, 2) for dp in range(DC // 2)],
                       lambda kc, tb: YB[:, kc, tb * 512:(tb + 1) * 512], [yreg], 4, evac, name="wo")


def load_xb(kb, XB, xreg, XF):
    for dc in range(DC):
        for hf in range(2):
            kb.dma("pool", XB[:, dc, hf * 1024:(hf + 1) * 1024], XF[dc][:, hf * 1024:(hf + 1) * 1024],
                   writes=[xreg[dc * 2 + hf]])


def odd_mixer(kb, XF, R, Wqkv, Wout, consts):
    nc = kb.nc
    with ExitStack() as es:
        YB = sbt(nc, es, "YB", [P, DC, S], BF16)
        yreg = Reg()
        with ExitStack() as esx:
            XB = sbt(nc, esx, "XB", [P, DC, S], BF16)
            xreg = [Reg() for _ in range(2 * DC)]
            load_xb(kb, XB, xreg, XF)
            attn_heads(kb, XB, xreg, YB, yreg, Wqkv, lambda h: (h * 128, 2048 + h * 128, 4096 + h * 128), 16, "moba", consts, 0)
            kb.barrier()
        out_proj(kb, XF, R, YB, yreg, Wout)
    kb.barrier()


def ssd_branch(kb, es_persist, Win, sp, li, consts, scr):
    nc = kb.nc
    es = es_persist
    xdt = sbt(nc, es, "xdt", [P, 16, 1024], BF16)
    BT = sbt(nc, es, "BT", [P, 4, S], BF16)
    CT = sbt(nc, es, "CT", [P, 4, S], BF16)
    acst = sbt(nc, es, "acst", [P, 16, 16], F32)
    xdtreg, btreg, ctreg, acreg = Reg(), Reg(), Reg(), Reg()
    xs_d, zs_d, acs_d = scr["xs_d"], scr["zs_d"], scr["acs_d"]

    def phase_a(XB, xreg):
        with ExitStack() as es2:
            phase_a_body(es2, XB, xreg)

    def phase_a_body(es2, XB, xreg):
        dtt = sbt(nc, es2, "dtt", [P, 16, 16], F32)
        dtreg = Reg()
        with ExitStack() as es3:
            wdt = sbt(nc, es3, "wdt", [P, DC, 16], BF16)
            dt = sbt(nc, es3, "dt", [16, S], F32)
            acs = sbt(nc, es3, "acs", [16, S], F32)
            one16 = sbt(nc, es3, "one16", [16, S], F32)
            ah = sbt(nc, es3, "ah", [16, 2], F32)
            r1 = Reg()
            wreg = Reg()
            with nc.allow_non_contiguous_dma(reason="dt weights"):
                kb.dma("pool", wdt[:], r3(Win)[:, :, 6144:6160], writes=[wreg])
            kb.op("pool", lambda e: e.memset(one16[:], 1.0), writes=[r1])
            kb.op("act", lambda e: e.activation(out=ah[:, 0:1], in_=sp.alog[:, li:li + 1], func=AF.Exp), reads=[consts.reg], writes=[r1])
            kb.op("dve", lambda e: e.tensor_scalar(out=ah[:, 1:2], in0=ah[:, 0:1], scalar1=-1.0, scalar2=None, op0=ALU.mult),
                  reads=[r1], writes=[r1])
            for tb in range(4):
                bt, br = kb.bank()
                for kc in range(DC):
                    kb.op("pe", lambda e: e.matmul(bt[0:16, :], lhsT=wdt[:, kc, :], rhs=XB[:, kc, tb * 512:(tb + 1) * 512],
                                                   start=(kc == 0), stop=(kc == DC - 1)), reads=[wreg] + xreg, writes=[br])
                sl = slice(tb * 512, (tb + 1) * 512)
                kb.op("act", lambda e: e.activation(out=dt[:, sl], in_=bt[0:16, :], func=AF.Exp, bias=sp.dtb[:, li:li + 1]),
                      reads=[br, consts.reg], writes=[r1])
            kb.op("dve", lambda e: e.tensor_scalar(out=dt[:], in0=dt[:], scalar1=1.0, scalar2=None, op0=ALU.add), reads=[r1], writes=[r1])
            kb.op("act", lambda e: e.activation(out=dt[:], in_=dt[:], func=AF.Ln), reads=[r1], writes=[r1])
            kb.op("dve", lambda e: e.tensor_scalar(out=acs[:], in0=dt[:], scalar1=ah[:, 1:2], scalar2=None, op0=ALU.mult),
                  reads=[r1], writes=[r1])
            kb.op("dve", lambda e: e.tensor_tensor_scan(out=acs[:], data0=one16[:], data1=acs[:], initial=0.0,
                                                        op0=ALU.mult, op1=ALU.add), reads=[r1], writes=[r1])
            kb.dma("sp", acs_d, acs[:], reads=[r1])
            for src, dst, dreg in ((dt, dtt, dtreg), (acs, acst, acreg)):
                bt, br = kb.bank()
                for tt in range(16):
                    kb.op("pe", lambda e: e.transpose(out=bt[:, tt * 16:(tt + 1) * 16], in_=src[:, tt * 128:(tt + 1) * 128],
                                                      identity=consts.ident_f[0:16, 0:16]), reads=[r1, consts.reg], writes=[br])
                kb.op("dve", lambda e: e.tensor_copy(out=dst[:].rearrange("p a b -> p (a b)"), in_=bt[:, 0:256]),
                      reads=[br], writes=[dreg])
            kb.barrier()
        with ExitStack() as es3:
            raw = [sbt(nc, es3, "raw%d" % i, [P, S], F32) for i in range(2)]
            rawreg = [Reg(), Reg()]
            cv = [sbt(nc, es3, "cv%d" % i, [P, S], F32) for i in range(2)]
            cvreg = [Reg(), Reg()]
            fb = [sbt(nc, es3, "fb%d" % i, [P, S], BF16) for i in range(2)]
            fbreg = [Reg(), Reg()]
            stg = [sbt(nc, es3, "stg%d" % i, [P, 4, P], BF16) for i in range(4)]
            stgreg = [Reg() for _ in range(4)]
            cnt = {"s": 0}

            def evac(ci, tb, bt, br):
                b = ci % 2
                sl = slice(tb * 512, (tb + 1) * 512)
                if ci < 8:
                    kb.op("act", lambda e: e.activation(out=fb[b][:, sl], in_=bt, func=AF.Silu), reads=[br], writes=[fbreg[b]])
                else:
                    kb.op("act", lambda e: e.activation(out=raw[b][:, sl], in_=bt, func=AF.Copy), reads=[br], writes=[rawreg[b]])
                if tb != 3:
                    return
                if ci >= 8:
                    c = ci - 8
                    cw = lambda k: sp.cw[:, li, c, k:k + 1]
                    kb.op("dve", lambda e: e.tensor_scalar(out=cv[b][:], in0=raw[b][:], scalar1=cw(3), scalar2=None, op0=ALU.mult),
                          reads=[rawreg[b], consts.reg], writes=[cvreg[b]])
                    for sh, k in ((1, 2), (2, 1), (3, 0)):
                        eng = "dve" if sh != 2 else "pool"
                        if eng == "dve":
                            kb.op("dve", lambda e: e.scalar_tensor_tensor(out=cv[b][:, sh:], in0=raw[b][:, 0:S - sh], scalar=cw(k),
                                                                          in1=cv[b][:, sh:], op0=ALU.mult, op1=ALU.add),
                                  reads=[rawreg[b], cvreg[b]], writes=[cvreg[b]])
                        else:
                            kb.op("dve", lambda e: e.scalar_tensor_tensor(out=cv[b][:, sh:], in0=raw[b][:, 0:S - sh], scalar=cw(k),
                                                                          in1=cv[b][:, sh:], op0=ALU.mult, op1=ALU.add),
                                  reads=[rawreg[b], cvreg[b]], writes=[cvreg[b]])
                    if c < 8:
                        dstb, dreg = fb[b][:], fbreg[b]
                    elif c < 12:
                        dstb, dreg = BT[:, c - 8, :], btreg
                    else:
                        dstb, dreg = CT[:, c - 12, :], ctreg
                    kb.op("act", lambda e: e.activation(out=dstb, in_=cv[b][:], func=AF.Silu, bias=sp.cb[:, li, c:c + 1]),
                          reads=[cvreg[b], consts.reg], writes=[dreg])
                    if c >= 8:
                        return
                isz = ci < 8
                c = ci if isz else ci - 8
                for t4 in range(4):
                    bt2, br2 = kb.bank()
                    bb = bt2.bitcast(BF16)
                    for j in range(4):
                        tt = t4 * 4 + j
                        kb.op("pe", lambda e: e.transpose(out=bb[:, j * 128:(j + 1) * 128], in_=fb[b][:, tt * 128:(tt + 1) * 128],
                                                          identity=consts.ident_b[:]), reads=[fbreg[b], consts.reg], writes=[br2])
                    i = cnt["s"] % 4
                    cnt["s"] += 1
                    src3 = bb[:, 0:512].rearrange("p (a b) -> p a b", b=128)
                    kb.op("act", lambda e: e.activation(out=stg[i][:], in_=src3, func=AF.Copy), reads=[br2], writes=[stgreg[i]])
                    if not isz:
                        for j in range(4):
                            tt = t4 * 4 + j
                            for hh in range(2):
                                kb.op("dve", lambda e: e.tensor_scalar(
                                    out=xdt[:, tt, c * 128 + hh * 64:c * 128 + (hh + 1) * 64],
                                    in0=bb[:, j * 128 + hh * 64:j * 128 + (hh + 1) * 64],
                                    scalar1=dtt[:, tt, 2 * c + hh:2 * c + hh + 1], scalar2=None, op0=ALU.mult),
                                    reads=[br2, dtreg], writes=[xdtreg])
                    dd = zs_d if isz else xs_d
                    kb.dma("sp", dd[t4 * 4:t4 * 4 + 4, :, c * 128:(c + 1) * 128].rearrange("a p f -> p a f"), stg[i][:],
                           reads=[stgreg[i]])

            chunks = [3072 + c * 128 for c in range(8)] + [4096 + c * 128 for c in range(16)]
            stream_gemm_fm(kb, es3, Win, DC, chunks, lambda kc, tb: XB[:, kc, tb * 512:(tb + 1) * 512], xreg, 4, evac,
                           slab=1, name="xbc")
        kb.barrier()

    def phase_b(YSd):
        with ExitStack() as es4:
            rows = sbt(nc, es4, "rows", [P, 4, S], F32)
            rowreg = [Reg() for _ in range(4)]
            E = [sbt(nc, es4, "E%d" % i, [P, S], F32) for i in range(2)]
            Ereg = [Reg(), Reg()]
            dg = [sbt(nc, es4, "dg%d" % i, [P, P], F32) for i in range(2)]
            dgreg = [Reg(), Reg()]
            pbuf = [sbt(nc, es4, "sp%d" % i, [P, S], BF16) for i in range(2)]
            pbreg = [Reg(), Reg()]
            pT = [sbt(nc, es4, "spT%d" % i, [P, 16, P], BF16) for i in range(2)]
            pTreg = [Reg(), Reg()]
            YS = sbt(nc, es4, "YS", [P, 8, S], BF16)
            ysreg = Reg()
            xz = [sbt(nc, es4, "xz%d" % i, [P, 2, 256], BF16) for i in range(2)]
            xzreg = [Reg(), Reg()]
            y1 = [sbt(nc, es4, "y1_%d" % i, [P, 256], F32) for i in range(2)]
            y1reg = [Reg(), Reg()]
            yb = [sbt(nc, es4, "yb_%d" % i, [P, 256], BF16) for i in range(2)]
            ybreg = [Reg(), Reg()]
            ss = sbt(nc, es4, "ss", [P, 4], F32)
            ssreg = Reg()
            sbanks = [kb.banks[i] for i in range(4)]
            sregs = [b[1] for b in sbanks]
            Sbig = kb.pbig[0]
            kb.bank_pool = [5, 6, 7]
            cnt = 0
            cg = 0
            for g in range(4):
                for hl in range(4):
                    h = g * 4 + hl
                    for hf in range(2):
                        kb.dma("sp", rows[:, hl, hf * 1024:(hf + 1) * 1024], acs_d[h, hf * 1024:(hf + 1) * 1024].partition_broadcast(P),
                               writes=[rowreg[hl]])
                for lt in range(16):
                    nk = (lt + 1) * 128
                    nch = (nk + 511) // 512
                    for c in range(nch):
                        w = min(512, nk - c * 512)
                        kb.op("pe", lambda e: e.matmul(sbanks[c][0][:, 0:w], lhsT=CT[:, g, lt * 128:(lt + 1) * 128],
                                                       rhs=BT[:, g, c * 512:c * 512 + w], start=True, stop=True),
                              reads=[ctreg, btreg], writes=[sregs[c]])
                    yt, yr = kb.banks[4]
                    for hl in range(4):
                        h = g * 4 + hl
                        i = cnt % 2
                        cnt += 1
                        al = acst[:, lt, h:h + 1]
                        d0 = lt * 128
                        kb.op("dve", lambda e: e.scalar_tensor_tensor(out=dg[i][:], in0=rows[:, hl, d0:d0 + 128], scalar=-1.0,
                                                                      in1=consts.trineg[:], op0=ALU.mult, op1=ALU.add),
                              reads=[rowreg[hl], consts.reg], writes=[dgreg[i]])
                        kb.op("act", lambda e: e.activation(out=E[i][:, d0:d0 + 128], in_=dg[i][:], func=AF.Exp, bias=al),
                              reads=[dgreg[i], acreg], writes=[Ereg[i]])
                        if lt > 0:
                            kb.op("act", lambda e: e.activation(out=E[i][:, 0:d0], in_=rows[:, hl, 0:d0], func=AF.Exp, scale=-1.0,
                                                                bias=al), reads=[rowreg[hl], acreg], writes=[Ereg[i]])
                        kb.op("dve", lambda e: e.tensor_tensor(out=pbuf[i][:, 0:nk], in0=Sbig[:, 0:nk], in1=E[i][:, 0:nk],
                                                               op=ALU.mult), reads=sregs[:nch] + [Ereg[i]], writes=[pbreg[i]])
                        nkt = lt + 1
                        for k4 in range((nkt + 3) // 4):
                            ks = list(range(k4 * 4, min(nkt, k4 * 4 + 4)))
                            transposes_bf16(kb, consts, [pbuf[i][:, k * 128:(k + 1) * 128] for k in ks], [pbreg[i]],
                                            pT[i][:, ks[0]:ks[-1] + 1, :], [pTreg[i]], "act" if k4 % 2 else "dve")
                        for k in range(nkt):
                            kb.op("pe", lambda e: e.matmul(yt[:, hl * 64:(hl + 1) * 64], lhsT=pT[i][:, k, :],
                                                           rhs=xdt[:, k, h * 64:(h + 1) * 64], start=(k == 0), stop=(k == nkt - 1)),
                                  reads=[pTreg[i], xdtreg], writes=[yr])
                    j = cg % 2
                    cg += 1
                    kb.dma("sp", xz[j][:, 0, :], xs_d[lt][:, g * 256:(g + 1) * 256], writes=[xzreg[j]])
                    kb.dma("sp", xz[j][:, 1, :], zs_d[lt][:, g * 256:(g + 1) * 256], writes=[xzreg[j]])
                    kb.op("dve", lambda e: e.tensor_tensor(out=y1[j][:], in0=xz[j][:, 0, :], in1=sp.dsk[:, g * 256:(g + 1) * 256],
                                                           op=ALU.mult), reads=[xzreg[j], consts.reg], writes=[y1reg[j]])
                    kb.op("dve", lambda e: e.tensor_tensor(out=y1[j][:], in0=y1[j][:], in1=yt[:, 0:256], op=ALU.add),
                          reads=[y1reg[j], yr], writes=[y1reg[j]])
                    kb.op("dve", lambda e: e.tensor_tensor(out=y1[j][:], in0=y1[j][:], in1=xz[j][:, 1, :], op=ALU.mult),
                          reads=[y1reg[j], xzreg[j]], writes=[y1reg[j]])
                    kb.op("dve", lambda e: e.tensor_tensor(out=yb[j][:], in0=y1[j][:], in1=y1[j][:], op=ALU.mult),
                          reads=[y1reg[j]], writes=[ybreg[j]])
                    kb.op("dve", lambda e: e.reduce_sum(out=ss[:, j:j + 1], in_=yb[j][:], axis=AX.X), reads=[ybreg[j]], writes=[ssreg])
                    kb.op("dve", lambda e: e.tensor_scalar(out=ss[:, j:j + 1], in0=ss[:, j:j + 1], scalar1=1.0 / 256, scalar2=RMS_EPS,
                                                           op0=ALU.mult, op1=ALU.add), reads=[ssreg], writes=[ssreg])
                    kb.op("act", lambda e: e.activation(out=ss[:, j:j + 1], in_=ss[:, j:j + 1], func=AF.Sqrt), reads=[ssreg], writes=[ssreg])
                    kb.op("dve", lambda e: e.reciprocal(out=ss[:, 2 + j:3 + j], in_=ss[:, j:j + 1]), reads=[ssreg], writes=[ssreg])
                    kb.op("dve", lambda e: e.scalar_tensor_tensor(out=yb[j][:], in0=y1[j][:], scalar=ss[:, 2 + j:3 + j],
                                                                  in1=sp.nw[:, g * 256:(g + 1) * 256], op0=ALU.mult, op1=ALU.mult),
                          reads=[y1reg[j], ssreg, consts.reg], writes=[ybreg[j]])
                    for q in range(2):
                        transposes_bf16(kb, consts, [yb[j][:, q * 128:(q + 1) * 128]], [ybreg[j]],
                                        YS[:, 2 * g + q:2 * g + q + 1, lt * 128:(lt + 1) * 128], [ysreg], "act")
            kb.bank_pool = list(range(8))
            for c in range(8):
                kb.dma("sp", YSd[c], YS[:, c, :], reads=[ysreg])
            kb.barrier()

    return phase_a, phase_b


def even_mixer(kb, XF, R, Win, Wout, consts, sp=None, li=0, scr=None):
    nc = kb.nc
    with ExitStack() as es:
        phase_a, phase_b = ssd_branch(kb, es, Win, sp, li, consts, scr)
        with ExitStack() as esx:
            XB = sbt(nc, esx, "XB", [P, DC, S], BF16)
            xreg = [Reg() for _ in range(2 * DC)]
            load_xb(kb, XB, xreg, XF)
            phase_a(XB, xreg)
            kb.barrier()
        phase_b(scr["yd"][8:16])
    kb.barrier()
    with ExitStack() as esx:
        XB = sbt(nc, esx, "XB", [P, DC, S], BF16)
        xreg = [Reg() for _ in range(2 * DC)]
        load_xb(kb, XB, xreg, XF)
        YA = sbt(nc, esx, "YA", [P, 8, S], BF16)
        yareg = Reg()
        attn_heads(kb, XB, xreg, YA, yareg, Win, lambda h: (h * 128, 1024 + h * 128, 2048 + h * 128), 8, "dil", consts, 0)
        for c in range(8):
            kb.dma("sp", scr["yd"][c], YA[:, c, :], reads=[yareg])
        kb.barrier()
    with ExitStack() as es:
        YB = sbt(nc, es, "YB", [P, DC, S], BF16)
        yreg = Reg()
        for c in range(DC):
            kb.dma("sp", YB[:, c, :], scr["yd"][c], writes=[yreg])
        out_proj(kb, XF, R, YB, yreg, Wout)
    kb.barrier()


def load_consts(kb, es, aps):
    nc = kb.nc
    c = Ctx()
    c.ones_f = sbt(nc, es, "ones_f", [P, P], F32)
    c.reg = Reg()
    kb.op("dve", lambda e: e.memset(c.ones_f[:], 1.0), writes=[c.reg])
    c.gb = sbt(nc, es, "gb", [P, 8, 2, DC], F32)
    kb.dma("sp", c.gb[:], aps["ln_gb"], writes=[c.reg])
    c.ident_b = sbt(nc, es, "ident_b", [P, P], BF16)
    kb.dma("pool", c.ident_b[:], aps["cst_ident"], writes=[c.reg])
    c.causal = sbt(nc, es, "causal", [P, 2, 256], BF16)
    kb.dma("pool", c.causal[:], aps["cst_causal"], writes=[c.reg])
    c.gdil = sbt(nc, es, "gdil", [P, 2048], BF16)
    kb.dma("pool", c.gdil[:], aps["cst_gdil"], writes=[c.reg])
    c.ident_f = sbt(nc, es, "ident_f", [P, P], F32)
    kb.dma("sp", c.ident_f[:], aps["cst_ident"], writes=[c.reg])
    c.trineg = sbt(nc, es, "trineg", [P, P], F32)
    kb.dma("sp", c.trineg[:], aps["cst_trineg"], writes=[c.reg])
    c.negmask = sbt(nc, es, "negmask", [P, 128], F32)
    kb.dma("sp", c.negmask[:], aps["cst_negmask"], writes=[c.reg])
    kb.barrier()
    return c


def build(stages, dbg=False):
    nc = bass.Bass("TRN2", target_bir_lowering=False)
    aps = {}

    def din(name, shape, dt=F32):
        aps[name] = nc.dram_tensor(name, list(shape), dt, kind="ExternalInput").ap()
        return aps[name]

    xT = din("xT", [DC, P, S])
    din("ln_gb", [P, 8, 2, DC])
    din("cst_ident", [P, P])
    din("cst_causal", [P, 2, 256])
    din("cst_gdil", [P, 2048])
    din("cst_negmask", [P, 128])
    din("cst_trineg", [P, P])
    kinds = set(st[0] for st in stages)
    if "oddmix" in kinds:
        din("od_w_qkv", [2, D, 3 * D])
        din("od_w_out", [2, D, D])
    if "evenmix" in kinds:
        din("ev_w_in", [2, D, EVEN_IN])
        din("ev_w_out", [2, D, D])
        din("ssd_cw", [P, 2, 16, 4])
        din("ssd_cb", [P, 2, 16])
        din("ssd_dtb", [16, 2])
        din("ssd_alog", [16, 2])
        din("ssd_dsk", [2, 1024])
        din("ssd_nw", [2, 1024])
    if "ffn" in kinds:
        din("ev_ffn_w_gu", [2, D, 2 * FF])
        din("ev_ffn_w_down", [2, FF, D])
    if "moe" in kinds:
        din("od_exp_w_gu", [2, NE, D, 2 * FF])
        din("od_exp_w_down", [2, NE, FF, D])
        din("rt_w", [P, 2, DC, NE])
        din("rt_b", [P, 2, NE])
    out = nc.dram_tensor("outT", [DC, P, S], F32, kind="ExternalOutput").ap()
    XF1 = nc.dram_tensor("XF1", [DC, P, S], F32, kind="Internal").ap()
    R = nc.dram_tensor("Rscr", [DC, P, S], F32, kind="Internal").ap()
    Gd = nc.dram_tensor("Gd", [NE, S], F32, kind="Internal").ap()
    with ExitStack() as es:
        kb = KB(nc, es)
        consts = load_consts(kb, es, aps)
        if "moe" in kinds:
            consts.rt_w = sbt(nc, es, "rt_w", [P, 2, DC, NE], F32)
            consts.rt_b = sbt(nc, es, "rt_b", [P, 2, NE], F32)
            kb.dma("sp", consts.rt_w[:], aps["rt_w"], writes=[consts.reg])
            kb.dma("sp", consts.rt_b[:], aps["rt_b"], writes=[consts.reg])
            kb.barrier()
        scr = None
        if "evenmix" in kinds:
            skind = "ExternalOutput" if os.environ.get("DBG_SSD") else "Internal"
            scr = {"xs_d": nc.dram_tensor("xs_d", [16, P, 1024], BF16, kind=skind).ap(),
                   "zs_d": nc.dram_tensor("zs_d", [16, P, 1024], BF16, kind=skind).ap(),
                   "acs_d": nc.dram_tensor("acs_d", [16, S], F32, kind=skind).ap(),
                   "yd": nc.dram_tensor("yd", [DC, P, S], BF16, kind=skind).ap()}
            sp = Ctx()
            sp.cw = sbt(nc, es, "ssd_cw", [P, 2, 16, 4], F32)
            sp.cb = sbt(nc, es, "ssd_cb", [P, 2, 16], F32)
            sp.dtb = sbt(nc, es, "ssd_dtb", [16, 2], F32)
            sp.alog = sbt(nc, es, "ssd_alog", [16, 2], F32)
            sp.dsk = sbt(nc, es, "ssd_dsk", [P, 1024], F32)
            sp.nw = sbt(nc, es, "ssd_nw", [P, 1024], F32)
            kb.dma("sp", sp.cw[:], aps["ssd_cw"], writes=[consts.reg])
            kb.dma("sp", sp.cb[:], aps["ssd_cb"], writes=[consts.reg])
            kb.dma("sp", sp.dtb[:], aps["ssd_dtb"], writes=[consts.reg])
            kb.dma("sp", sp.alog[:], aps["ssd_alog"], writes=[consts.reg])
            kb.barrier()
        cur = xT
        for si, st in enumerate(stages):
            kind, i, slot = st[0], st[1], st[2]
            last = si == len(stages) - 1
            if kind == "oddmix":
                odd_mixer(kb, cur, R, aps["od_w_qkv"][i], aps["od_w_out"][i], consts)
            elif kind == "evenmix":
                kb.dma("sp", sp.dsk[:], aps["ssd_dsk"][i].partition_broadcast(P), writes=[consts.reg])
                kb.dma("sp", sp.nw[:], aps["ssd_nw"][i].partition_broadcast(P), writes=[consts.reg])
                kb.barrier()
                even_mixer(kb, cur, R, aps["ev_w_in"][i], aps["ev_w_out"][i], consts, sp=sp, li=i, scr=scr)
            elif kind == "ffn":
                ffn_dense(kb, cur, R, aps["ev_ffn_w_gu"][i], aps["ev_ffn_w_down"][i], consts)
            elif kind == "moe":
                moe_dense(kb, cur, R, Gd, aps["od_exp_w_gu"][i], aps["od_exp_w_down"][i], consts.rt_w[:, i], consts.rt_b[:, i],
                          consts)
            ln_phase(kb, R, out if last else XF1, consts.gb, slot, consts)
            cur = XF1
        kb.barrier()
    return nc


def prep_ln(inputs):
    arr = np.zeros((P, 8, 2, DC), np.float32)
    for layer in range(DEPTH):
        i = layer // 2
        pre = "ev" if layer % 2 == 0 else "od"
        for j, nm in enumerate(["ln1", "ln2"]):
            g = np.asarray(inputs["%s_%s_g" % (pre, nm)][i], np.float32).reshape(DC, P).T
            b = np.asarray(inputs["%s_%s_b" % (pre, nm)][i], np.float32).reshape(DC, P).T
            arr[:, layer * 2 + j, 0, :] = g
            arr[:, layer * 2 + j, 1, :] = b
    return arr


def make_consts():
    c = {}
    c["cst_ident"] = np.eye(P, dtype=np.float32)
    ql = np.arange(P)[:, None]
    kl = np.arange(256)[None, :]
    causal = np.zeros((P, 2, 256), np.float32)
    causal[:, 0, :] = (kl <= ql)
    causal[:, 1, :] = (kl <= ql + 128)
    c["cst_causal"] = causal
    u = np.arange(2048)[None, :]
    delta = 1920 + ql - u
    mult = ((delta >= 0) & (delta <= 128)).astype(np.float32)
    mult += ((delta >= 0) & (delta % 4 == 0) & (delta // 4 <= 128)).astype(np.float32)
    mult += ((delta >= 0) & (delta % 16 == 0) & (delta // 16 <= 128)).astype(np.float32)
    c["cst_gdil"] = mult.astype(np.float32)
    nm = np.zeros((P, 16, 8), np.float32)
    for qt in range(16):
        for j in range(8):
            if j >= qt // 2:
                nm[:, qt, j] = -1e30
    c["cst_negmask"] = nm.reshape(P, 128)
    sl = np.arange(P)[None, :]
    c["cst_trineg"] = np.where(sl <= ql, 0.0, -30000.0).astype(np.float32)
    return c


def prep_ssd(inputs):
    o = {}
    cw = np.asarray(inputs["ev_conv_w"], np.float32)
    o["ssd_cw"] = np.ascontiguousarray(cw.reshape(2, 4, 16, P).transpose(3, 0, 2, 1))
    cb = np.asarray(inputs["ev_conv_b"], np.float32)
    o["ssd_cb"] = np.ascontiguousarray(cb.reshape(2, 16, P).transpose(2, 0, 1))
    o["ssd_dtb"] = np.ascontiguousarray(np.asarray(inputs["ev_dt_bias"], np.float32).T)
    o["ssd_alog"] = np.ascontiguousarray(np.asarray(inputs["ev_a_log"], np.float32).T)
    o["ssd_dsk"] = np.ascontiguousarray(np.repeat(np.asarray(inputs["ev_d_skip"], np.float32), 64, axis=1))
    o["ssd_nw"] = np.ascontiguousarray(np.asarray(inputs["ev_ssd_norm"], np.float32))
    return o


FULL_STAGES = [("evenmix", 0, 0), ("ffn", 0, 1), ("oddmix", 0, 2), ("moe", 0, 3),
               ("evenmix", 1, 4), ("ffn", 1, 5), ("oddmix", 1, 6), ("moe", 1, 7)]


def kernel(**inputs):
    x = np.asarray(inputs["x"], np.float32)
    nc = build(FULL_STAGES)
    shared = make_consts()
    shared["ln_gb"] = prep_ln(inputs)
    shared.update(prep_ssd(inputs))
    for k in ["od_w_qkv", "od_w_out", "ev_w_in", "ev_w_out", "ev_ffn_w_gu", "ev_ffn_w_down", "od_exp_w_gu", "od_exp_w_down"]:
        shared[k] = np.ascontiguousarray(np.asarray(inputs[k], np.float32))
    rw = np.asarray(inputs["od_router_w"], np.float32)
    shared["rt_w"] = np.ascontiguousarray(rw.reshape(2, DC, P, NE).transpose(2, 0, 1, 3))
    rb = np.asarray(inputs["od_router_b"], np.float32)
    shared["rt_b"] = np.ascontiguousarray(np.broadcast_to(rb[None], (P, 2, NE)))
    in_maps = []
    for c in range(NCORES):
        m = dict(shared)
        m["xT"] = np.ascontiguousarray(x[c].T).reshape(DC, P, S)
        in_maps.append(m)
    res = run_bass_kernel_spmd(nc, in_maps, core_ids=list(range(NCORES)))
    out = np.stack([r["outT"].reshape(D, S).T for r in res.results], axis=0)
    return np.ascontiguousarray(out.astype(np.float32))
```
